# Optimizing a Trainium2 kernel written in Bass

```python
import math
import jax, jax.numpy as jnp
from jax import lax
import numpy as np

D_MODEL = 2048
BATCH = 16
SEQ = 256
DEPTH = 2
DEC_BATCH = 2
DEC_SEQ = 1024
PAST_LEN = 256

GRID_W = 64
HEAD_DIM = 128
N_Q_HEADS = 8
N_KV_HEADS = 2
Q_GROUP = N_Q_HEADS // N_KV_HEADS
Q_WIDTH = N_Q_HEADS * HEAD_DIM
KV_WIDTH = N_KV_HEADS * HEAD_DIM
Q_BLOCK = 128
ROPE_THETA = 10000.0
LRU_WIDTH = D_MODEL // 2
LRU_BLOCKS = 8
LRU_BLOCK = LRU_WIDTH // LRU_BLOCKS
LRU_CONV_W = 4
LRU_C = 8.0
MIX0_IN = Q_WIDTH + 2 * KV_WIDTH + 2 * LRU_WIDTH
MIX0_OUT = Q_WIDTH + LRU_WIDTH
HYENA_ORDER = 2
HYENA_WIDTH = D_MODEL
HYENA_SHORT_W = 3
HYENA_EMB = 33
HYENA_BANDS = (HYENA_EMB - 1) // 2
HYENA_FILTER_HIDDEN = 64
D_FF = 5632
N_EXPERTS = 8
TOP_K = 2
D_FF_EXPERT = 7168
N_MOD = 6
LN_EPS = 1e-5
QK_EPS = 1e-6
DN_ALPHA = (2 * DEPTH) ** 0.25
DN_BETA = (8 * DEPTH) ** -0.25

kernel_name = 'hybrid_diffusion_prefix_step'


def layer_norm(x, g, b):
    xf = x.astype(jnp.float32)
    mu = jnp.mean(xf, -1, keepdims=True)
    var = jnp.mean(jnp.square(xf - mu), -1, keepdims=True)
    return ((xf - mu) * lax.rsqrt(var + LN_EPS) * g + b).astype(x.dtype)


def rms_norm(x, g):
    xf = x.astype(jnp.float32)
    return (xf * lax.rsqrt(jnp.mean(xf * xf, -1, keepdims=True) + QK_EPS) * g).astype(x.dtype)


def ada_modulation(cond, w, b):
    m = jnp.einsum('nd,de->ne', jax.nn.silu(cond), w) + b
    return [t[:, None, :] for t in jnp.split(m, N_MOD, axis=-1)]


def modulate(x, shift, scale):
    return x * (1 + scale) + shift


def post_norm(x, delta, gate, g, b):
    return layer_norm(DN_ALPHA * x + gate * delta, g, b)


def depthwise_conv(x, w, b):
    width = w.shape[0]
    left = (width - 1) // 2
    y = lax.conv_general_dilated(x, w[:, None, :].astype(x.dtype), window_strides=(1,),
                                 padding=[(left, width - 1 - left)],
                                 dimension_numbers=('NWC', 'WIO', 'NWC'),
                                 feature_group_count=x.shape[-1])
    return y + b


def axial_rope(x):
    L = x.shape[1]
    t = jnp.arange(L)
    row = (t // GRID_W).astype(jnp.float32)
    col = (t % GRID_W).astype(jnp.float32)
    n_freq = HEAD_DIM // 4
    inv = 1.0 / (ROPE_THETA ** (jnp.arange(n_freq, dtype=jnp.float32) / n_freq))
    ang = jnp.concatenate([row[:, None] * inv, col[:, None] * inv], -1)
    cos = jnp.cos(ang)[None, :, None, :]
    sin = jnp.sin(ang)[None, :, None, :]
    xf = x.astype(jnp.float32).reshape(x.shape[:-1] + (HEAD_DIM // 2, 2))
    x0, x1 = xf[..., 0], xf[..., 1]
    out = jnp.stack([x0 * cos - x1 * sin, x0 * sin + x1 * cos], -1)
    return out.reshape(x.shape).astype(x.dtype)


def block_attention(q, k, v):
    B, Lq, _, hd = q.shape
    qb = q.reshape(B, Lq // Q_BLOCK, Q_BLOCK, N_KV_HEADS, Q_GROUP, hd).transpose(1, 0, 2, 3, 4, 5)
    scale = hd ** -0.5

    def one_block(q_blk):
        s = jnp.einsum('bqhgd,bkhd->bhgqk', q_blk, k, preferred_element_type=jnp.float32) * scale
        p = jax.nn.softmax(s, axis=-1).astype(v.dtype)
        return jnp.einsum('bhgqk,bkhd->bqhgd', p, v)

    o = lax.map(one_block, qb)
    return o.transpose(1, 0, 2, 3, 4, 5).reshape(B, Lq, N_Q_HEADS * hd)


def block_diag_linear(x, w, b):
    xb = x.reshape(x.shape[:-1] + (LRU_BLOCKS, LRU_BLOCK))
    return jnp.einsum('blhi,hij->blhj', xb, w).reshape(x.shape) + b


def _linear_combine(e1, e2):
    a1, b1 = e1
    a2, b2 = e2
    return a1 * a2, a2 * b1 + b2


def rglru(x, lam, w_r, b_r, w_i, b_i, h0, reverse):
    if reverse:
        x = jnp.flip(x, 1)
    r = jax.nn.sigmoid(block_diag_linear(x, w_r, b_r).astype(jnp.float32))
    i = jax.nn.sigmoid(block_diag_linear(x, w_i, b_i).astype(jnp.float32))
    log_a = -LRU_C * r * jax.nn.softplus(-lam.astype(jnp.float32))
    a = jnp.exp(log_a)
    b = jnp.sqrt(-jnp.expm1(2.0 * log_a)) * (i * x.astype(jnp.float32))
    b = b.at[:, 0].add(a[:, 0] * h0.astype(jnp.float32))
    _, h = lax.associative_scan(_linear_combine, (a, b), axis=1)
    return jnp.flip(h, 1) if reverse else h


def _ab_project(u, p):
    B, L, _ = u.shape
    proj = jnp.einsum('bld,de->ble', u, p['w_in'])
    q, k, v, xb, gb = jnp.split(proj, [Q_WIDTH, Q_WIDTH + KV_WIDTH, Q_WIDTH + 2 * KV_WIDTH,
                                      Q_WIDTH + 2 * KV_WIDTH + LRU_WIDTH], axis=-1)
    q = rms_norm(q.reshape(B, L, N_Q_HEADS, HEAD_DIM), p['q_norm'])
    k = rms_norm(k.reshape(B, L, N_KV_HEADS, HEAD_DIM), p['k_norm'])
    v = v.reshape(B, L, N_KV_HEADS, HEAD_DIM)
    xc = depthwise_conv(xb, p['conv_w'], p['conv_b'])
    return q, k, v, xc, gb


def _lru_bidir(xc, gb, p, h0):
    hf = rglru(xc, p['lam'][0], p['w_r'][0], p['b_r'][0], p['w_i'][0], p['b_i'][0], h0[:, 0], False)
    hb = rglru(xc, p['lam'][1], p['w_r'][1], p['b_r'][1], p['w_i'][1], p['b_i'][1], h0[:, 1], True)
    y = (hf + hb).astype(xc.dtype) * jax.nn.gelu(gb)
    return y, jnp.stack([hf[:, -1], hb[:, 0]], axis=1)


def mixer_ab_context(u, p):
    q, k, v, xc, gb = _ab_project(u, p)
    attn = block_attention(q, k, v)
    h0 = jnp.zeros((u.shape[0], 2, LRU_WIDTH), jnp.float32)
    lru, h_fin = _lru_bidir(xc, gb, p, h0)
    out = jnp.einsum('ble,ed->bld', jnp.concatenate([attn, lru], -1), p['w_out'])
    return out, k, v, h_fin


def mixer_ab_latent(u, p, ctx_k, ctx_v, ctx_h):
    q, k, v, xc, gb = _ab_project(u, p)
    q, k = axial_rope(q), axial_rope(k)
    attn = block_attention(q, jnp.concatenate([ctx_k, k], 1), jnp.concatenate([ctx_v, v], 1))
    lru, _ = _lru_bidir(xc, gb, p, ctx_h)
    return jnp.einsum('ble,ed->bld', jnp.concatenate([attn, lru], -1), p['w_out'])


def hyena_kernel_fft(L, p):
    t = jnp.arange(L, dtype=jnp.float32)
    t01 = t / (L - 1)
    w = 2.0 * math.pi * t / L
    f = jnp.linspace(1e-4, HYENA_BANDS - 1, HYENA_BANDS, dtype=jnp.float32)
    fw = w[:, None] * f[None, :]
    feat = jnp.concatenate([t01[:, None], jnp.cos(fw), -jnp.sin(fw)], -1)
    h = jnp.sin(p['filt_f1'] * (feat @ p['filt_w1'] + p['filt_b1']))
    h = jnp.sin(p['filt_f2'] * (h @ p['filt_w2'] + p['filt_b2']))
    h = (h @ p['filt_w3']).reshape(L, 2, HYENA_ORDER, HYENA_WIDTH)
    h = h * jnp.exp(-t01[:, None, None, None] * jnp.abs(p['filt_decay']))
    zero = jnp.zeros((1, HYENA_ORDER, HYENA_WIDTH), h.dtype)
    k2 = jnp.concatenate([h[:, 0], zero, jnp.flip(h[1:, 1], 0)], 0)
    return jnp.fft.rfft(k2.astype(jnp.float32), axis=0)


def fft_long_conv(u, k_f, bias):
    L = u.shape[1]
    uf = u.astype(jnp.float32)
    y = jnp.fft.irfft(jnp.fft.rfft(uf, n=2 * L, axis=1) * k_f, n=2 * L, axis=1)[:, :L]
    return (y + uf * bias).astype(u.dtype)


def mixer_hyena(u, p):
    L = u.shape[1]
    proj = depthwise_conv(jnp.einsum('bld,de->ble', u, p['w_in']), p['short_w'], p['short_b'])
    x1, x2, z = jnp.split(proj, 3, axis=-1)
    k_f = hyena_kernel_fft(L, p)
    for n, gate in enumerate((x1, x2)):
        z = gate * fft_long_conv(z, k_f[:, n], p['filt_bias'][n])
    return jnp.einsum('ble,ed->bld', z, p['w_out'])


def swiglu(u, p):
    h = jax.nn.silu(u @ p['ffn_w1']) * (u @ p['ffn_w3'])
    return h @ p['ffn_w2']


def moe_swiglu(u, p):
    logits = jnp.einsum('bld,de->ble', u, p['router']).astype(jnp.float32)
    top_v, top_i = lax.top_k(logits, TOP_K)
    probs = jax.nn.softmax(top_v, axis=-1)
    combine = jnp.sum(jax.nn.one_hot(top_i, N_EXPERTS, dtype=jnp.float32) * probs[..., None], axis=-2)
    out = jnp.zeros(u.shape, jnp.float32)
    for e in range(N_EXPERTS):
        h = jax.nn.silu(u @ p['exp_w1'][e]) * (u @ p['exp_w3'][e])
        out = out + combine[..., e:e + 1] * (h @ p['exp_w2'][e])
    return out.astype(u.dtype)


def setup_inputs(seed: int = 0) -> dict:
    key = jax.random.key(seed)
    ks = iter(jax.random.split(key, 64))
    f32 = jnp.float32
    D = D_MODEL

    def nrm(shape, scale):
        return jax.random.normal(next(ks), shape, f32) * scale

    def gain(n):
        return 1.0 + nrm((n,), 0.02)

    a8 = jax.random.uniform(next(ks), (2, LRU_WIDTH), f32, 0.9, 0.999)
    a = a8 ** (1.0 / LRU_C)
    lru_lambda = jnp.log(a) - jnp.log1p(-a)
    decay = jax.random.uniform(next(ks), (2, HYENA_ORDER, HYENA_WIDTH), f32,
                               math.log(100.0) / 1.5, math.log(100.0) / 0.3)
    return {
        'x_prompt': nrm((BATCH, SEQ, D), 1.0),
        'x_sample': nrm((DEC_BATCH, DEC_SEQ, D), 1.0),
        'c': nrm((DEC_BATCH, D), 1.0),
        'c_ctx': nrm((D,), 1.0),
        'cache_l0_k': nrm((DEC_BATCH, PAST_LEN, N_KV_HEADS, HEAD_DIM), 1.0),
        'cache_l0_v': nrm((DEC_BATCH, PAST_LEN, N_KV_HEADS, HEAD_DIM), 1.0),
        'state_l0_lru': nrm((DEC_BATCH, 2, LRU_WIDTH), 0.5),
        'l0_ada_w': nrm((D, N_MOD * D), D ** -0.5),
        'l0_ada_b': nrm((N_MOD * D,), 0.02),
        'l0_w_in': nrm((D, MIX0_IN), D ** -0.5),
        'l0_q_norm': gain(HEAD_DIM),
        'l0_k_norm': gain(HEAD_DIM),
        'l0_lru_conv_w': nrm((LRU_CONV_W, LRU_WIDTH), LRU_CONV_W ** -0.5),
        'l0_lru_conv_b': nrm((LRU_WIDTH,), 0.02),
        'l0_lru_lambda': lru_lambda,
        'l0_lru_w_r': nrm((2, LRU_BLOCKS, LRU_BLOCK, LRU_BLOCK), LRU_BLOCK ** -0.5),
        'l0_lru_b_r': nrm((2, LRU_WIDTH), 0.02),
        'l0_lru_w_i': nrm((2, LRU_BLOCKS, LRU_BLOCK, LRU_BLOCK), LRU_BLOCK ** -0.5),
        'l0_lru_b_i': nrm((2, LRU_WIDTH), 0.02),
        'l0_w_out': nrm((MIX0_OUT, D), MIX0_OUT ** -0.5 * DN_BETA),
        'l0_ln1_g': gain(D),
        'l0_ln1_b': nrm((D,), 0.02),
        'l0_ffn_w1': nrm((D, D_FF), D ** -0.5),
        'l0_ffn_w3': nrm((D, D_FF), D ** -0.5),
        'l0_ffn_w2': nrm((D_FF, D), D_FF ** -0.5 * DN_BETA),
        'l0_ln2_g': gain(D),
        'l0_ln2_b': nrm((D,), 0.02),
        'l1_ada_w': nrm((D, N_MOD * D), D ** -0.5),
        'l1_ada_b': nrm((N_MOD * D,), 0.02),
        'l1_w_in': nrm((D, 3 * HYENA_WIDTH), D ** -0.5),
        'l1_short_w': nrm((HYENA_SHORT_W, 3 * HYENA_WIDTH), HYENA_SHORT_W ** -0.5),
        'l1_short_b': nrm((3 * HYENA_WIDTH,), 0.02),
        'l1_filt_w1': nrm((HYENA_EMB, HYENA_FILTER_HIDDEN), HYENA_EMB ** -0.5),
        'l1_filt_b1': nrm((HYENA_FILTER_HIDDEN,), 0.02),
        'l1_filt_f1': gain(HYENA_FILTER_HIDDEN),
        'l1_filt_w2': nrm((HYENA_FILTER_HIDDEN, HYENA_FILTER_HIDDEN), HYENA_FILTER_HIDDEN ** -0.5),
        'l1_filt_b2': nrm((HYENA_FILTER_HIDDEN,), 0.02),
        'l1_filt_f2': gain(HYENA_FILTER_HIDDEN),
        'l1_filt_w3': nrm((HYENA_FILTER_HIDDEN, 2 * HYENA_ORDER * HYENA_WIDTH), 0.1 * HYENA_FILTER_HIDDEN ** -0.5),
        'l1_filt_decay': decay,
        'l1_filt_bias': nrm((HYENA_ORDER, HYENA_WIDTH), 0.5),
        'l1_w_out': nrm((HYENA_WIDTH, D), HYENA_WIDTH ** -0.5 * DN_BETA),
        'l1_ln1_g': gain(D),
        'l1_ln1_b': nrm((D,), 0.02),
        'l1_router': nrm((D, N_EXPERTS), D ** -0.5),
        'l1_exp_w1': nrm((N_EXPERTS, D, D_FF_EXPERT), D ** -0.5),
        'l1_exp_w3': nrm((N_EXPERTS, D, D_FF_EXPERT), D ** -0.5),
        'l1_exp_w2': nrm((N_EXPERTS, D_FF_EXPERT, D), D_FF_EXPERT ** -0.5 * DN_BETA),
        'l1_ln2_g': gain(D),
        'l1_ln2_b': nrm((D,), 0.02),
    }


def reference(x_prompt, x_sample, c, c_ctx, cache_l0_k, cache_l0_v, state_l0_lru,
              l0_ada_w, l0_ada_b, l0_w_in, l0_q_norm, l0_k_norm, l0_lru_conv_w, l0_lru_conv_b,
              l0_lru_lambda, l0_lru_w_r, l0_lru_b_r, l0_lru_w_i, l0_lru_b_i, l0_w_out,
              l0_ln1_g, l0_ln1_b, l0_ffn_w1, l0_ffn_w3, l0_ffn_w2, l0_ln2_g, l0_ln2_b,
              l1_ada_w, l1_ada_b, l1_w_in, l1_short_w, l1_short_b,
              l1_filt_w1, l1_filt_b1, l1_filt_f1, l1_filt_w2, l1_filt_b2, l1_filt_f2, l1_filt_w3,
              l1_filt_decay, l1_filt_bias, l1_w_out, l1_ln1_g, l1_ln1_b,
              l1_router, l1_exp_w1, l1_exp_w3, l1_exp_w2, l1_ln2_g, l1_ln2_b):
    params = [
        dict(ada_w=l0_ada_w, ada_b=l0_ada_b, w_in=l0_w_in, q_norm=l0_q_norm, k_norm=l0_k_norm,
             conv_w=l0_lru_conv_w, conv_b=l0_lru_conv_b, lam=l0_lru_lambda,
             w_r=l0_lru_w_r, b_r=l0_lru_b_r, w_i=l0_lru_w_i, b_i=l0_lru_b_i, w_out=l0_w_out,
             ln1_g=l0_ln1_g, ln1_b=l0_ln1_b, ffn_w1=l0_ffn_w1, ffn_w3=l0_ffn_w3, ffn_w2=l0_ffn_w2,
             ln2_g=l0_ln2_g, ln2_b=l0_ln2_b),
        dict(ada_w=l1_ada_w, ada_b=l1_ada_b, w_in=l1_w_in, short_w=l1_short_w, short_b=l1_short_b,
             filt_w1=l1_filt_w1, filt_b1=l1_filt_b1, filt_f1=l1_filt_f1,
             filt_w2=l1_filt_w2, filt_b2=l1_filt_b2, filt_f2=l1_filt_f2, filt_w3=l1_filt_w3,
             filt_decay=l1_filt_decay, filt_bias=l1_filt_bias, w_out=l1_w_out,
             ln1_g=l1_ln1_g, ln1_b=l1_ln1_b, router=l1_router,
             exp_w1=l1_exp_w1, exp_w3=l1_exp_w3, exp_w2=l1_exp_w2,
             ln2_g=l1_ln2_g, ln2_b=l1_ln2_b),
    ]
    context_cache = [(cache_l0_k, cache_l0_v, state_l0_lru), None]
    cond_ctx = c_ctx[None, :]
    xp, xs = x_prompt, x_sample
    new_k = new_v = new_h = None
    for layer in range(DEPTH):
        p = params[layer]
        mp = ada_modulation(cond_ctx, p['ada_w'], p['ada_b'])
        ms = ada_modulation(c, p['ada_w'], p['ada_b'])
        up, us = modulate(xp, mp[0], mp[1]), modulate(xs, ms[0], ms[1])
        if layer % 2 == 0:
            ck, cv, ch = context_cache[layer]
            dp, new_k, new_v, new_h = mixer_ab_context(up, p)
            ds = mixer_ab_latent(us, p, ck, cv, ch)
        else:
            dp, ds = mixer_hyena(up, p), mixer_hyena(us, p)
        xp = post_norm(xp, dp, mp[2], p['ln1_g'], p['ln1_b'])
        xs = post_norm(xs, ds, ms[2], p['ln1_g'], p['ln1_b'])
        up, us = modulate(xp, mp[3], mp[4]), modulate(xs, ms[3], ms[4])
        if layer % 2 == 0:
            dp, ds = swiglu(up, p), swiglu(us, p)
        else:
            dp, ds = moe_swiglu(up, p), moe_swiglu(us, p)
        xp = post_norm(xp, dp, mp[5], p['ln2_g'], p['ln2_b'])
        xs = post_norm(xs, ds, ms[5], p['ln2_g'], p['ln2_b'])
    return (xp, xs, new_k, new_v, new_h)
```

```python
import math
import numpy as np
import concourse.bass as bass
import concourse.mybir as mybir
from concourse.bass_utils import run_bass_kernel_spmd

F32 = mybir.dt.float32
BF16 = mybir.dt.bfloat16
AF = mybir.ActivationFunctionType
ALU = mybir.AluOpType
AX = mybir.AxisListType


class Sched:
    ENG = ('pe', 'act', 'dve', 'pool', 'sp')

    def __init__(self):
        self.ops = []
        self.lastw = {}
        self.readers = {}
        self.cnt = {e: 0 for e in self.ENG}
        self.chan_cnt = {}
        self.chan_eng = {}
        self.bar_pending = {}
        self.branches = []
        self.cur = None

    def op(self, eng, fn, r=(), w=(), chan=None, ndma=1):
        deps = set()
        for k in r:
            if k in self.lastw:
                deps.add(self.lastw[k])
        for k in w:
            if k in self.lastw:
                deps.add(self.lastw[k])
            deps.update(self.readers.get(k, ()))
        deps |= self.bar_pending.pop(eng, set())
        if chan is None:
            self.cnt[eng] += 1
            sig = ('e', eng, self.cnt[eng])
        else:
            self.chan_cnt[chan] = self.chan_cnt.get(chan, 0) + 16 * ndma
            self.chan_eng[chan] = eng
            sig = ('c', chan, self.chan_cnt[chan])
        self.ops.append(dict(eng=eng, fn=fn, deps=deps, sig=sig, chan=chan, br=self.cur))
        for k in r:
            self.readers.setdefault(k, []).append(sig)
        for k in w:
            self.lastw[k] = sig
            self.readers[k] = []
        return sig

    def _all_signals(self):
        s = {('e', e, v) for e, v in self.cnt.items() if v > 0}
        s |= {('c', c, v) for c, v in self.chan_cnt.items() if v > 0}
        return s

    def barrier(self):
        sig = self._all_signals()
        for e in self.ENG:
            self.bar_pending[e] = set(sig)
        self.lastw = {}
        self.readers = {}

    def branch_begin(self, cond_fn):
        assert self.cur is None
        self.barrier()
        b = dict(cond_fn=cond_fn, base_cnt=dict(self.cnt), base_chan=dict(self.chan_cnt),
                 bar=self._all_signals(), ends=[])
        self.branches.append(b)
        self.cur = (len(self.branches) - 1, 0)

    def _end_path(self):
        b = self.branches[self.cur[0]]
        b['ends'].append((dict(self.cnt), dict(self.chan_cnt)))

    def branch_next(self):
        self._end_path()
        bi, pi = self.cur
        b = self.branches[bi]
        self.cnt = dict(b['base_cnt'])
        self.chan_cnt = dict(b['base_chan'])
        self.lastw, self.readers = {}, {}
        self.bar_pending = {e: set(b['bar']) for e in self.ENG}
        self.cur = (bi, pi + 1)

    def branch_end(self):
        self._end_path()
        b = self.branches[self.cur[0]]
        cnt = {}
        for e in self.ENG:
            cnt[e] = max(c[e] for c, _ in b['ends'])
        chans = set()
        for _, cc in b['ends']:
            chans |= set(cc)
        chan = {c: max(cc.get(c, 0) for _, cc in b['ends']) for c in chans}
        b['final_cnt'], b['final_chan'] = cnt, chan
        self.cnt, self.chan_cnt = dict(cnt), dict(chan)
        self.cur = None
        self.barrier()

    def finish(self):
        self.barrier()
        self.op('sp', None)

    def emit(self, eng_name, eng, sems):
        waited = {}

        def emit_op(o):
            for (kind, key, val) in sorted(o['deps']):
                if eng_name == 'pe' and kind == 'e' and key == 'pe':
                    continue
                if waited.get((kind, key), 0) >= val:
                    continue
                eng.wait_ge(sems[(kind, key)], val)
                waited[(kind, key)] = val
            if o['fn'] is None:
                return
            res = o['fn'](eng)
            if o['chan'] is not None:
                for ins in res:
                    ins.then_inc(sems[('c', o['chan'])], 16)
            else:
                res.then_inc(sems[('e', eng_name)], 1)

        def pad(b, pi):
            end_cnt, end_chan = b['ends'][pi]
            need = b['final_cnt'][eng_name] - end_cnt[eng_name]
            if need > 0:
                eng.wait_ge(sems[('e', eng_name)], end_cnt[eng_name])
                eng.sem_inc(sems[('e', eng_name)], need)
            for c, fin in b['final_chan'].items():
                if self.chan_eng.get(c) != eng_name:
                    continue
                have = end_chan.get(c, 0)
                if fin - have > 0:
                    eng.wait_ge(sems[('c', c)], have)
                    eng.sem_inc(sems[('c', c)], fin - have)

        mine = [o for o in self.ops if o['eng'] == eng_name]
        done_br = set()
        for o in mine:
            if o['br'] is None:
                emit_op(o)
                continue
            bi = o['br'][0]
            if bi in done_br:
                continue
            done_br.add(bi)
            b = self.branches[bi]
            npaths = len(b['ends'])
            w0 = dict(waited)
            for (kind, key, val) in sorted(b['bar']):
                if waited.get((kind, key), 0) < val and not (eng_name == 'pe' and kind == 'e' and key == 'pe'):
                    eng.wait_ge(sems[(kind, key)], val)
                    waited[(kind, key)] = val
            w1 = dict(waited)
            cond_ap, thrs = b['cond_fn']
            if not isinstance(thrs, (list, tuple)):
                thrs = [thrs]
            assert npaths == len(thrs) + 1

            def emit_path(pi):
                waited.clear()
                waited.update(w1)
                for oo in mine:
                    if oo['br'] == (bi, pi):
                        emit_op(oo)
                pad(b, pi)

            def nest(pi):
                if pi == npaths - 1:
                    emit_path(pi)
                    return
                with eng.If_lt(cr, thrs[pi]):
                    emit_path(pi)
                with eng.Else():
                    nest(pi + 1)
            with eng.register("br%d_%s" % (bi, eng_name)) as cr:
                eng.reg_load(cr, cond_ap)
                nest(0)
            waited.clear()
            waited.update(w1)


class Arena:
    def __init__(self, ap_f32, nwords):
        self.ap = ap_f32
        self.n = nwords
        self.off = 0
        self.mark = 0

    def alloc(self, free_shape, dtype=F32):
        if isinstance(free_shape, int):
            free_shape = (free_shape,)
        n = int(np.prod(free_shape))
        words = n if dtype == F32 else (n + 1) // 2
        assert self.off + words <= self.n, ("arena overflow", self.off, words, self.n)
        a = self.ap[:, self.off:self.off + words]
        self.off += words
        if dtype != F32:
            a = a.bitcast(dtype)
        if len(free_shape) == 2:
            a = a.rearrange("p (a b) -> p a b", b=free_shape[1])
        elif len(free_shape) == 3:
            a = a.rearrange("p (a b c) -> p a b c", b=free_shape[1], c=free_shape[2])
        return a

    def at(self, off, free_shape, dtype=F32):
        save = self.off
        self.off = off
        a = self.alloc(free_shape, dtype)
        self.off = save
        return a

    def set_mark(self):
        self.mark = self.off

    def reset(self):
        self.off = self.mark


D = 2048
NCH = 16
LP, LS = 256, 1024
NCOL = 1536
NMOE = 768
DFF = 5632
DFFE = 7168
NEXP = 8
ALPHA = 4.0 ** 0.25
LN_EPS = 1e-5
QK_EPS = 1e-6
ARENA_WORDS = 47104
MAGIC = 12582912.0

PK = {}
_o = 0
for _n, _w in [('ada_b0', 96), ('ada_b1', 96), ('ln', 128), ('conv_w', 32), ('conv_b', 8), ('lam', 16),
               ('b_r', 16), ('b_i', 16), ('h0', 16), ('short_w', 144), ('short_b', 48), ('fbias', 32),
               ('qmask', 4)]:
    PK[_n] = (_o, _w)
    _o += _w
NPK = _o


class Ctx:
    pass


def build_program(debug=False):
    nc = bass.Bass("TRN2", target_bir_lowering=False)
    K = Ctx()
    K.nc = nc
    S = K.S = Sched()

    def din(name, shape):
        return nc.dram_tensor(name, list(shape), F32, kind="ExternalInput").ap()

    def dout(name, shape):
        return nc.dram_tensor(name, list(shape), F32, kind="ExternalOutput").ap()

    def dscr(name, shape):
        return nc.dram_tensor(name, list(shape), F32, kind="ExternalOutput" if debug else "Internal").ap()

    I = {}
    for name, shape in [
        ('xT', (128, NCH, NCOL)), ('condT', (128, NCH, 2)), ('pk', (128, NPK)), ('qg', (128, 128)), ('kg', (128, 128)),
        ('rope', (128, 8, 2, 64)), ('ckT', (128, 2, 256)), ('cv', (128, 2, 256)),
        ('l0_ada_w', (24, 128, NCH, 512)), ('l0_w_in', (7, 128, NCH, 512)), ('lru_wr', (128, 2, 8, 128)), ('lru_wi', (128, 2, 8, 128)),
        ('l0_w_out', (4, 128, NCH, 512)), ('l0_ffn_w1', (22, 128, NCH, 256)), ('l0_ffn_w3', (22, 128, NCH, 256)), ('l0_ffn_w2', (DFF, D)),
        ('l1_ada_w', (24, 128, NCH, 512)), ('l1_w_in', (48, 128, NCH, 128)), ('l1_w_out', (4, 128, NCH, 512)),
        ('router', (128, NCH, 8)), ('exp_w1', (NEXP, 28, 128, NCH, 256)), ('exp_w3', (NEXP, 28, 128, NCH, 256)), ('exp_w2', (NEXP, DFFE, D)),
        ('featT256', (33, 256)), ('featT1024', (33, 1024)), ('fw1', (33, 64)), ('fw2', (64, 64)), ('fw3', (64, 8192)),
        ('pk64', (64, 4)), ('decay', (1, 8192)), ('nt01_256', (128, 2)), ('nt01_1024', (128, 8)),
        ('dft256', (5, 256, 256)), ('dft1024', (5, 1024, 1024)),
        ('ident', (128, 128)), ('sel', (8, 8 * 128)), ('tri', (128, 128)), ('iota', (128, 512)),
    ]:
        I[name] = din(name, shape)
    O = {}
    O['yT'] = dout('yT', (128, NCH, NMOE))
    O['kout'] = dout('kout', (512, 256))
    O['vout'] = dout('vout', (512, 256))
    O['hout'] = dout('hout', (128, 32))
    XS1 = dscr('XS1', (128, NCH, NCOL))
    XS2 = dscr('XS2', (128, NCH, NCOL))
    XS3 = dscr('XS3', (128, NCH, NCOL))
    MO = dscr('MO', (128, NCH, NMOE))
    KF = {256: dscr('KF256', (3, 2, 128, 2 * D)), 1024: dscr('KF1024', (3, 8, 128, 2 * D))}
    ZZ = dscr('ZZ', (128, NCH, NCOL))

    import contextlib
    st = contextlib.ExitStack()
    arena_t = st.enter_context(nc.sbuf_tensor("arena", [128, ARENA_WORDS], F32))
    ps = [st.enter_context(nc.psum_tensor("ps%d" % i, [128, 512], F32))[:] for i in range(8)]
    psb = [p.bitcast(BF16) for p in ps]
    A = Arena(arena_t[:], ARENA_WORDS)

    pk = A.alloc(NPK)
    mod = [A.alloc((96, 2)), A.alloc((96, 2))]
    ident = A.alloc(128, BF16)
    ones = A.alloc(128, BF16)
    c8 = A.alloc(16)
    hst = A.alloc(32)
    A.set_mark()

    def pkc(name, i=0, n=1):
        o, w = PK[name]
        return pk[:, o + i:o + i + n]

    def modc(l, part, c, cond):
        return mod[l][:, part * 16 + c, cond:cond + 1]

    uid = [0]

    def U(prefix):
        uid[0] += 1
        return "%s#%d" % (prefix, uid[0])

    class WS:
        def __init__(self, name, nslots, free_shape):
            self.name, self.n, self.i = name, nslots, 0
            self.slots = [A.alloc(free_shape, BF16) for _ in range(nslots)]

        def load(self, src_ap, sub=None):
            s = self.i % self.n
            self.i += 1
            dst = self.slots[s] if sub is None else sub(self.slots[s])
            key = "%s_%d" % (self.name, s)
            S.op('pool', lambda e: [e.dma_start(out=dst, in_=src_ap, max_dma_last_dim=8192)], w=[key], chan=key)
            return self.slots[s], key

    def wunit(w_ap, c0, ncols):
        assert c0 % ncols == 0 and w_ap.shape[-1] == ncols, (w_ap.shape, c0, ncols)
        return w_ap[c0 // ncols]

    def mm_group(out_ps, lhs_list, rhs_list, r, w):
        n = len(lhs_list)

        def fn(e):
            ins = None
            for i in range(n):
                ins = e.matmul(out_ps, lhs_list[i], rhs_list[i], start=(i == 0), stop=(i == n - 1))
            return ins
        S.op('pe', fn, r=r, w=w)

    S.op('sp', lambda e: [e.dma_start(out=pk, in_=I['pk'])], w=['pk'], chan='ld_pk')
    S.op('pool', lambda e: [e.dma_start(out=ident, in_=I['ident'])], w=['ident'], chan='ld_ident')
    S.op('dve', lambda e: e.memset(ones, 1.0), w=['ones'])
    S.op('act', lambda e: e.activation(c8, pkc('lam', 0, 16), AF.Exp, scale=-1.0), r=['pk'], w=['c8'])
    S.op('act', lambda e: e.activation(c8, c8, AF.Ln, bias=1.0), r=['c8'], w=['c8'])
    S.op('dve', lambda e: e.tensor_scalar(c8, c8, -8.0, None, ALU.mult), r=['c8'], w=['c8'])

    def phase_ada():
        condT = A.alloc((NCH, 2))
        sT = A.alloc((NCH, 2), BF16)
        sg = A.alloc((NCH, 2))
        S.op('sp', lambda e: [e.dma_start(out=condT, in_=I['condT'])], w=['condT'], chan='ld_cond')
        S.op('act', lambda e: e.activation(sg, condT, AF.Silu), r=['condT'], w=['sg'])
        S.op('dve', lambda e: e.tensor_copy(sT, sg), r=['sg'], w=['sT'])
        ws = WS('wa', 3, (NCH, 512))
        for l in range(2):
            wname = 'l%d_ada_w' % l
            for u in range(24):
                slot, key = ws.load(wunit(I[wname], u * 512, 512))
                bank = ps[u % 2]
                bk = 'ps%d' % (u % 2)
                for m in range(4):
                    mm_group(bank[:, m * 2:m * 2 + 2], [slot[:, k, m * 128:(m + 1) * 128] for k in range(NCH)],
                             [sT[:, k, :] for k in range(NCH)], r=[key, 'sT'], w=[bk])
                c0 = u * 4
                bo = PK['ada_b%d' % l][0]
                S.op('dve', lambda e, bank=bank, l=l, c0=c0, bo=bo: e.tensor_tensor(
                    mod[l][:, c0:c0 + 4, :], bank[:, 0:8].rearrange("p (a b) -> p a b", b=2),
                    pk[:, bo + c0:bo + c0 + 4].unsqueeze(2).to_broadcast([128, 4, 2]), ALU.add),
                    r=[bk, 'pk'], w=['mod%d' % l])
            for part in (1, 4):
                S.op('dve', lambda e, l=l, part=part: e.tensor_scalar(
                    mod[l][:, part * 16:(part + 1) * 16, :], mod[l][:, part * 16:(part + 1) * 16, :], 1.0, None, ALU.add),
                    r=['mod%d' % l], w=['mod%d' % l])

    phase_ada()
    S.barrier()
    A.reset()

    K.__dict__.update(dict(I=I, O=O, A=A, ps=ps, psb=psb, pk=pk, mod=mod, ident=ident, ones=ones, c8=c8, hst=hst,
                           pkc=pkc, modc=modc, WS=WS, wunit=wunit, mm_group=mm_group, XS1=XS1, XS2=XS2, XS3=XS3,
                           MO=MO, KF=KF, ZZ=ZZ, st=st, U=U))
    return K


def load_x_tile(K, dst, src, c0, n, key, chan):
    K.S.op('sp', lambda e: [e.dma_start(out=dst, in_=src[:, :, c0:c0 + n])], w=[key], chan=chan)


def modulate_to_bf16(K, l, pshift, pscale, cond_of_col, xt, xkey, uT, ukey, c0s):
    S = K.S
    for c in range(NCH):
        for (a, n, cond) in c0s:
            S.op('dve', lambda e, c=c, a=a, n=n, cond=cond: e.tensor_scalar(
                uT[:, c, a:a + n], xt[:, c, a:a + n], K.modc(l, pscale, c, cond), K.modc(l, pshift, c, cond),
                ALU.mult, ALU.add), r=[xkey, 'mod'], w=[ukey])


def post_norm(K, l, pgate, ln_idx, xt, xkey, delta_fn, conds, ncols, tmpA):
    S = K.S
    ps = K.ps
    tmp = tmpA.alloc(ncols)
    yb = [tmpA.alloc(ncols, BF16) for _ in range(2)]
    ysq = [tmpA.alloc(ncols, BF16) for _ in range(2)]
    mean = tmpA.alloc(ncols)
    rstd = tmpA.alloc(ncols)
    k_tmp, k_mean, k_rstd = 'pn_tmp', 'pn_mean', 'pn_rstd'
    k_yb = ['pn_yb0', 'pn_yb1']
    k_ysq = ['pn_ysq0', 'pn_ysq1']
    for m in range(NCH):
        dap, dkey = delta_fn(m)
        for (a, n, cond) in conds:
            S.op('act', lambda e, a=a, n=n, cond=cond, dap=dap, m=m: e.activation(
                tmp[:, a:a + n], dap[:, a:a + n], AF.Identity, scale=K.modc(l, pgate, m, cond)),
                r=[dkey, 'mod'], w=[k_tmp])
        S.op('dve', lambda e, m=m: e.scalar_tensor_tensor(xt[:, m, 0:ncols], xt[:, m, 0:ncols], ALPHA, tmp[:, 0:ncols],
                                                          ALU.mult, ALU.add), r=[k_tmp, xkey], w=[xkey])
        b = m % 2
        S.op('act', lambda e, m=m, b=b: e.activation(ysq[b], xt[:, m, 0:ncols], AF.Square), r=[xkey], w=[k_ysq[b]])
        S.op('dve', lambda e, m=m, b=b: e.tensor_copy(yb[b], xt[:, m, 0:ncols]), r=[xkey], w=[k_yb[b]])
        S.op('pe', lambda e, m=m, b=b: e.matmul(ps[6][:, 0:ncols], K.ones, yb[b], start=(m == 0), stop=(m == NCH - 1)),
             r=[k_yb[b], 'ones'], w=['ps6'])
        S.op('pe', lambda e, m=m, b=b: e.matmul(ps[7][:, 0:ncols], K.ones, ysq[b], start=(m == 0), stop=(m == NCH - 1)),
             r=[k_ysq[b], 'ones'], w=['ps7'])
    S.op('dve', lambda e: e.tensor_scalar(mean, ps[6][:, 0:ncols], 1.0 / D, None, ALU.mult), r=['ps6'], w=[k_mean])
    S.op('dve', lambda e: e.tensor_tensor(tmp, mean, mean, ALU.mult), r=[k_mean], w=[k_tmp])
    S.op('dve', lambda e: e.scalar_tensor_tensor(rstd, ps[7][:, 0:ncols], 1.0 / D, tmp, ALU.mult, ALU.subtract),
         r=['ps7', k_tmp], w=[k_rstd])
    S.op('act', lambda e: e.activation(rstd, rstd, AF.Sqrt, bias=LN_EPS), r=[k_rstd], w=[k_rstd])
    S.op('dve', lambda e: e.reciprocal(rstd, rstd), r=[k_rstd], w=[k_rstd])
    go = PK['ln'][0] + ln_idx * 32
    for m in range(NCH):
        S.op('dve', lambda e, m=m: e.tensor_tensor(xt[:, m, 0:ncols], xt[:, m, 0:ncols], mean, ALU.subtract),
             r=[xkey, k_mean], w=[xkey])
        S.op('dve', lambda e, m=m: e.tensor_tensor(xt[:, m, 0:ncols], xt[:, m, 0:ncols], rstd, ALU.mult),
             r=[xkey, k_rstd], w=[xkey])
        S.op('dve', lambda e, m=m: e.tensor_scalar(xt[:, m, 0:ncols], xt[:, m, 0:ncols], K.pk[:, go + m:go + m + 1],
                                                   K.pk[:, go + 16 + m:go + 17 + m], ALU.mult, ALU.add),
             r=[xkey, 'pk'], w=[xkey])


def wout_postnorm(K, l, w_ap, catT, catkey, col0, ncols, conds, src, dst, ws, gate_part, ln_idx, tagp, xt=None):
    S, A, ps = K.S, K.A, K.ps
    if xt is None:
        xt = A.alloc((NCH, ncols))
    xkey = 'xt'
    load_x_tile(K, xt, src, col0, ncols, xkey, 'ld_x_' + tagp)
    state = {}

    def delta_fn(m):
        u, mm = divmod(m, 4)
        if mm == 0:
            state['slot'], state['key'] = ws.load(K.wunit(w_ap, u * 512, 512))
        slot, key = state['slot'], state['key']
        bank = ps[m % 2]
        K.mm_group(bank[:, 0:ncols], [slot[:, k, mm * 128:(mm + 1) * 128] for k in range(NCH)],
                   [catT[:, k, col0:col0 + ncols] for k in range(NCH)], r=[key, catkey], w=['ps%d' % (m % 2)])
        return bank, 'ps%d' % (m % 2)
    post_norm(K, l, gate_part, ln_idx, xt, xkey, delta_fn, conds, ncols, A)
    S.op('sp', lambda e: [e.dma_start(out=dst[:, :, col0:col0 + ncols], in_=xt)], r=[xkey], chan='st_x_' + tagp)


def phase_mixer0(K, grp):
    S, A, I, O, ps, psb = K.S, K.A, K.I, K.O, K.ps, K.psb
    if grp == 'P':
        col0, ncols, nseg, L, cond, koff = 0, 512, 2, 256, 0, 0
    else:
        col0, ncols, nseg, L, cond, koff = 512, 1024, 1, 1024, 1, 256
    ntb = ncols // 128
    nkb_seg = (L + koff) // 128
    conds = [(0, ncols, cond)]
    uT_off = A.off
    uT = A.alloc((NCH, ncols), BF16)
    catT = A.alloc((NCH, ncols), BF16)
    qg = A.alloc(128)
    kg = A.alloc(128)
    wr = A.alloc((2, 8, 128), BF16)
    wi = A.alloc((2, 8, 128), BF16)
    S.op('sp', lambda e: [e.dma_start(out=qg, in_=I['qg'])], w=['qg'], chan='ld_qg')
    S.op('sp', lambda e: [e.dma_start(out=kg, in_=I['kg'])], w=['kg'], chan='ld_kg')
    S.op('pool', lambda e: [e.dma_start(out=wr, in_=I['lru_wr'])], w=['wr'], chan='ld_wr')
    S.op('pool', lambda e: [e.dma_start(out=wi, in_=I['lru_wi'])], w=['wi'], chan='ld_wi')
    if grp == 'S':
        rope = A.alloc((8, 2, 64))
        S.op('sp', lambda e: [e.dma_start(out=rope, in_=I['rope'])], w=['rope'], chan='ld_rope')
    ws = K.WS('wm', 2, (NCH, 512))
    base = A.off
    xtmp = A.alloc((NCH, 512))
    for t in range(ncols // 512):
        load_x_tile(K, xtmp, I['xT'], col0 + t * 512, 512, 'xtmp', 'ld_xtmp')
        for c in range(NCH):
            S.op('dve', lambda e, c=c, t=t: e.tensor_scalar(
                uT[:, c, t * 512:(t + 1) * 512], xtmp[:, c, :], K.modc(0, 1, c, cond), K.modc(0, 0, c, cond),
                ALU.mult, ALU.add), r=['xtmp', 'mod'], w=['uT'])
    S.barrier()
    A.off = base
    qT = A.alloc((8, ncols), BF16)
    kT = A.alloc((2, koff + ncols), BF16)
    V = A.alloc((koff // 128 + ntb, 256), BF16)
    if grp == 'S':
        S.op('pool', lambda e: [e.dma_start(out=kT[:, :, 0:256], in_=I['ckT'])], w=['kT'], chan='ld_ck')
        S.op('pool', lambda e: [e.dma_start(out=V[:, 0:2, :], in_=I['cv'])], w=['V'], chan='ld_cv')
    qf = A.alloc(512)
    sq = A.alloc(512)
    qr = A.alloc(512)
    ss = A.alloc(4)
    qb = A.alloc(512, BF16)
    kn = [A.alloc(256), A.alloc(256)]
    vf = [A.alloc(256), A.alloc(256)]

    def norm_heads(nh, gain, gkey, dst_f32):
        S.op('dve', lambda e: e.tensor_tensor(sq[:, 0:nh * 128], qf[:, 0:nh * 128], qf[:, 0:nh * 128], ALU.mult),
             r=['qf'], w=['sq'])
        S.op('dve', lambda e: e.tensor_reduce(ss[:, 0:nh], sq[:, 0:nh * 128].rearrange("p (a b) -> p a b", b=128),
                                              AX.X, ALU.add), r=['sq'], w=['ss'])
        S.op('act', lambda e: e.activation(ss[:, 0:nh], ss[:, 0:nh], AF.Sqrt, scale=1.0 / 128, bias=QK_EPS),
             r=['ss'], w=['ss'])
        S.op('dve', lambda e: e.reciprocal(ss[:, 0:nh], ss[:, 0:nh]), r=['ss'], w=['ss'])
        for h in range(nh):
            S.op('dve', lambda e, h=h: e.scalar_tensor_tensor(
                dst_f32[:, h * 128:(h + 1) * 128], qf[:, h * 128:(h + 1) * 128], ss[:, h:h + 1], gain,
                ALU.mult, ALU.mult), r=['qf', 'ss', gkey], w=['qn'])

    def rope_to_bf16(nh, src_f32, tb):
        if grp == 'P':
            S.op('dve', lambda e: e.tensor_copy(qb[:, 0:nh * 128], src_f32[:, 0:nh * 128]), r=['qn'], w=['qb'])
            return
        cos = rope[:, tb, 0, :]
        sin = rope[:, tb, 1, :]
        sv = src_f32[:, 0:nh * 128].rearrange("p (h i two) -> p h i two", i=64, two=2)
        qv = qb[:, 0:nh * 128].rearrange("p (h i two) -> p h i two", i=64, two=2)
        t0 = sq[:, 0:nh * 64].rearrange("p (h i) -> p h i", i=64)
        t1 = sq[:, 256:256 + nh * 64].rearrange("p (h i) -> p h i", i=64)
        cb = cos.unsqueeze(1).to_broadcast([128, nh, 64])
        sb = sin.unsqueeze(1).to_broadcast([128, nh, 64])
        x0 = sv[:, :, :, 0]
        x1 = sv[:, :, :, 1]
        S.op('dve', lambda e: e.tensor_tensor(t0, x0, cb, ALU.mult), r=['qn', 'rope'], w=['sq'])
        S.op('dve', lambda e: e.tensor_tensor(t1, x1, sb, ALU.mult), r=['qn', 'rope'], w=['sq'])
        S.op('dve', lambda e: e.tensor_tensor(qv[:, :, :, 0], t0, t1, ALU.subtract), r=['sq'], w=['qb'])
        S.op('dve', lambda e: e.tensor_tensor(t0, x0, sb, ALU.mult), r=['qn', 'rope', 'qb'], w=['sq'])
        S.op('dve', lambda e: e.tensor_tensor(t1, x1, cb, ALU.mult), r=['qn', 'rope'], w=['sq'])
        S.op('dve', lambda e: e.tensor_tensor(qv[:, :, :, 1], t0, t1, ALU.add), r=['sq'], w=['qb'])

    def transpose_heads(nh, dstT, h0, c0, dkey):
        def fn(e):
            ins = None
            for h in range(nh):
                ins = e.transpose(psb[6][:, h * 128:(h + 1) * 128], qb[:, h * 128:(h + 1) * 128], K.ident)
            return ins
        S.op('pe', fn, r=['qb', 'ident'], w=['ps6'])
        S.op('act', lambda e: e.activation(dstT[:, h0:h0 + nh, c0:c0 + 128],
                                           psb[6][:, 0:nh * 128].rearrange("p (h t) -> p h t", t=128), AF.Copy),
             r=['ps6'], w=[dkey])

    for u in range(3):
        slot, wkey = ws.load(K.wunit(I['l0_w_in'], u * 512, 512))
        for tb in range(ntb):
            bank, bk = ps[tb % 2], 'ps%d' % (tb % 2)
            K.mm_group(bank, [uT[:, k, tb * 128:(tb + 1) * 128] for k in range(NCH)], [slot[:, k, :] for k in range(NCH)],
                       r=[wkey, 'uT'], w=[bk])
            if u < 2:
                S.op('act', lambda e, bank=bank: e.activation(qf, bank, AF.Copy), r=[bk], w=['qf'])
                norm_heads(4, qg, 'qg', qr)
                rope_to_bf16(4, qr, tb)
                transpose_heads(4, qT, 4 * u, tb * 128, 'qT')
            else:
                kb = tb % 2
                S.op('act', lambda e, bank=bank: e.activation(qf[:, 0:256], bank[:, 0:256], AF.Copy), r=[bk], w=['qf'])
                S.op('act', lambda e, bank=bank, kb=kb: e.activation(vf[kb], bank[:, 256:512], AF.Copy), r=[bk],
                     w=['vf%d' % kb])
                norm_heads(2, kg, 'kg', kn[kb])
                rope_to_bf16(2, kn[kb], tb)
                transpose_heads(2, kT, 0, koff + tb * 128, 'kT')
                S.op('dve', lambda e, kb=kb, tb=tb: e.tensor_copy(V[:, koff // 128 + tb, :], vf[kb]),
                     r=['vf%d' % kb], w=['V'])
                if grp == 'P':
                    S.op('sp', lambda e, kb=kb, tb=tb: [e.dma_start(out=O['kout'][tb * 128:(tb + 1) * 128, :], in_=kn[kb])],
                         r=['qn'], chan='st_k%d' % kb)
                    S.op('sp', lambda e, kb=kb, tb=tb: [e.dma_start(out=O['vout'][tb * 128:(tb + 1) * 128, :], in_=vf[kb])],
                         r=['vf%d' % kb], chan='st_v%d' % kb)
    Eb = [A.alloc(512, BF16), A.alloc(512, BF16)]
    rden = A.alloc(512)
    it = 0
    sc = 128.0 ** -0.5
    for seg in range(nseg):
        kbase = seg * L if grp == 'P' else 0
        vbase = seg * (L // 128) if grp == 'P' else 0
        for h2 in range(2):
            for qb_i in range(L // 128):
                qc = seg * L + qb_i * 128
                rhs_q = qT[:, 4 * h2:4 * h2 + 4, qc:qc + 128]
                for kb in range(nkb_seg):
                    sb_, sk = ps[2 + it % 2], 'ps%d' % (2 + it % 2)
                    eb, ek = Eb[it % 2], 'E%d' % (it % 2)
                    it += 1
                    S.op('pe', lambda e, sb_=sb_, kb=kb, h2=h2, rhs_q=rhs_q, kbase=kbase: e.matmul(
                        sb_, kT[:, h2, kbase + kb * 128:kbase + (kb + 1) * 128], rhs_q, start=True, stop=True),
                        r=['kT', 'qT'], w=[sk])
                    S.op('act', lambda e, sb_=sb_, eb=eb: e.activation(eb, sb_, AF.Exp, scale=sc), r=[sk], w=[ek])
                    first, last = kb == 0, kb == nkb_seg - 1
                    S.op('pe', lambda e, eb=eb, kb=kb, h2=h2, first=first, last=last, vbase=vbase: e.matmul(
                        ps[4], V[:, vbase + kb, h2 * 128:(h2 + 1) * 128], eb, start=first, stop=last),
                        r=[ek, 'V'], w=['ps4'])
                    S.op('pe', lambda e, eb=eb, first=first, last=last: e.matmul(ps[5], K.ones, eb, start=first, stop=last),
                         r=[ek, 'ones'], w=['ps5'])
                S.op('dve', lambda e: e.reciprocal(rden, ps[5]), r=['ps5'], w=['rden'])
                S.op('dve', lambda e, h2=h2, qc=qc: e.tensor_tensor(
                    catT[:, 4 * h2:4 * h2 + 4, qc:qc + 128], ps[4].rearrange("p (h t) -> p h t", t=128),
                    rden.rearrange("p (h t) -> p h t", t=128), ALU.mult), r=['ps4', 'rden'], w=['catT'])
    S.barrier()
    A.off = base
    pb = A.alloc((nseg, L + 3))
    xc = A.alloc(ncols)
    xcb = A.alloc(ncols, BF16)
    gbf = A.alloc(ncols)
    t1 = A.alloc(ncols)
    t2 = A.alloc(ncols)
    aa = A.alloc(ncols)
    bb = A.alloc(ncols)
    hh = [A.alloc(ncols), A.alloc(ncols)]
    nb = ncols // 512
    S.op('dve', lambda e: e.memset(pb, 0.0), w=['pb'])
    cwo, cbo = PK['conv_w'][0], PK['conv_b'][0]
    seg3 = lambda ap: ap.rearrange("p (s l) -> p s l", l=L)
    for uu in range(2):
        sx, kx = ws.load(K.wunit(I['l0_w_in'], 1536 + uu * 512, 512))
        sg_, kg_ = ws.load(K.wunit(I['l0_w_in'], 2560 + uu * 512, 512))
        for jj in range(4):
            j = uu * 4 + jj
            for b in range(nb):
                K.mm_group(ps[b], [sx[:, k, jj * 128:(jj + 1) * 128] for k in range(NCH)],
                           [uT[:, k, b * 512:(b + 1) * 512] for k in range(NCH)], r=[kx, 'uT'], w=['ps%d' % b])
                K.mm_group(ps[2 + b], [sg_[:, k, jj * 128:(jj + 1) * 128] for k in range(NCH)],
                           [uT[:, k, b * 512:(b + 1) * 512] for k in range(NCH)], r=[kg_, 'uT'], w=['ps%d' % (2 + b)])
                if grp == 'P':
                    S.op('act', lambda e, b=b: e.activation(pb[:, :, 1:L + 1], seg3(ps[b]), AF.Copy), r=['ps%d' % b], w=['pb'])
                else:
                    S.op('act', lambda e, b=b: e.activation(pb[:, 0, 1 + b * 512:1 + (b + 1) * 512], ps[b], AF.Copy),
                         r=['ps%d' % b], w=['pb'])
                S.op('act', lambda e, b=b: e.activation(gbf[:, b * 512:(b + 1) * 512], ps[2 + b], AF.Copy),
                     r=['ps%d' % (2 + b)], w=['gbf'])
            xc3 = seg3(xc)
            S.op('dve', lambda e, j=j: e.tensor_scalar(xc3, pb[:, :, 0:L], K.pk[:, cwo + j * 4:cwo + j * 4 + 1],
                                                       K.pk[:, cbo + j:cbo + j + 1], ALU.mult, ALU.add),
                 r=['pb', 'pk'], w=['xc'])
            for k in range(1, 4):
                S.op('dve', lambda e, j=j, k=k: e.scalar_tensor_tensor(
                    xc3, pb[:, :, k:k + L], K.pk[:, cwo + j * 4 + k:cwo + j * 4 + k + 1], xc3, ALU.mult, ALU.add),
                    r=['pb', 'pk', 'xc'], w=['xc'])
            S.op('dve', lambda e: e.tensor_copy(xcb, xc), r=['xc'], w=['xcb'])
            for d in range(2):
                for b in range(nb):
                    S.op('pe', lambda e, d=d, j=j, b=b: e.matmul(ps[4 + b], wr[:, d, j, :], xcb[:, b * 512:(b + 1) * 512],
                                                                  start=True, stop=True), r=['wr', 'xcb'], w=['ps%d' % (4 + b)])
                    S.op('pe', lambda e, d=d, j=j, b=b: e.matmul(ps[6 + b], wi[:, d, j, :], xcb[:, b * 512:(b + 1) * 512],
                                                                  start=True, stop=True), r=['wi', 'xcb'], w=['ps%d' % (6 + b)])
                    bs = slice(b * 512, (b + 1) * 512)
                    S.op('act', lambda e, d=d, j=j, b=b, bs=bs: e.activation(
                        t1[:, bs], ps[4 + b], AF.Sigmoid, bias=K.pkc('b_r', d * 8 + j)), r=['ps%d' % (4 + b), 'pk'], w=['t1'])
                    S.op('act', lambda e, d=d, j=j, b=b, bs=bs: e.activation(
                        t2[:, bs], ps[6 + b], AF.Sigmoid, bias=K.pkc('b_i', d * 8 + j)), r=['ps%d' % (6 + b), 'pk'], w=['t2'])
                S.op('act', lambda e, d=d, j=j: e.activation(aa, t1, AF.Exp, scale=K.c8[:, d * 8 + j:d * 8 + j + 1]),
                     r=['t1', 'c8'], w=['aa'])
                S.op('dve', lambda e: e.tensor_tensor(t1, aa, aa, ALU.mult), r=['aa'], w=['t1'])
                S.op('act', lambda e: e.activation(t1, t1, AF.Sqrt, scale=-1.0, bias=1.0), r=['t1'], w=['t1'])
                S.op('dve', lambda e: e.tensor_tensor(t2, t2, xc, ALU.mult), r=['t2', 'xc'], w=['t2'])
                S.op('dve', lambda e: e.tensor_tensor(bb, t1, t2, ALU.mult), r=['t1', 't2'], w=['bb'])
                for seg in range(nseg):
                    sl = slice(seg * L, (seg + 1) * L)
                    init = 0.0 if grp == 'P' else K.pkc('h0', d * 8 + j)
                    if d == 0:
                        S.op('dve', lambda e, sl=sl, init=init: e.tensor_tensor_scan(hh[0][:, sl], aa[:, sl], bb[:, sl], init,
                                                                                     ALU.mult, ALU.add),
                             r=['aa', 'bb', 'pk'], w=['hh0'])
                    else:
                        S.op('dve', lambda e, sl=sl, init=init: e.tensor_tensor_scan(
                            hh[1][:, sl][:, ::-1], aa[:, sl][:, ::-1], bb[:, sl][:, ::-1], init, ALU.mult, ALU.add),
                            r=['aa', 'bb', 'pk'], w=['hh1'])
                    if grp == 'P':
                        hc = seg * 16 + d * 8 + j
                        pos = seg * L + (L - 1 if d == 0 else 0)
                        S.op('act', lambda e, hc=hc, pos=pos, d=d: e.activation(K.hst[:, hc:hc + 1], hh[d][:, pos:pos + 1], AF.Copy),
                             r=['hh%d' % d], w=['hst'])
            S.op('act', lambda e: e.activation(gbf, gbf, AF.Gelu_apprx_tanh), r=['gbf'], w=['gbf'])
            S.op('dve', lambda e: e.tensor_tensor(hh[0], hh[0], hh[1], ALU.add), r=['hh0', 'hh1'], w=['hh0'])
            S.op('dve', lambda e, j=j: e.tensor_tensor(catT[:, 8 + j, :], hh[0], gbf, ALU.mult), r=['hh0', 'gbf'], w=['catT'])
    if grp == 'P':
        S.op('sp', lambda e: [e.dma_start(out=O['hout'], in_=K.hst)], r=['hst'], chan='st_h')
    S.barrier()
    for t in range(ncols // 512):
        A.off = base
        xt_pre = A.at(uT_off, (NCH, 512)) if grp == 'S' else None
        cnd = [(0, 512, cond)]
        wout_postnorm(K, 0, I['l0_w_out'], catT, 'catT', t * 512, 512, cnd, _ColShift(I['xT'], col0), _ColShift(K.XS1, col0),
                      ws, 2, 0, 'm0', xt=xt_pre)


class _ColShift:
    def __init__(self, ap, off):
        self.ap, self.off = ap, off

    def __getitem__(self, idx):
        p, c, s = idx
        return self.ap[p, c, slice(s.start + self.off, s.stop + self.off)]


def ffn_pass(K, uT, ukey, halves, w1_ap, w3_ap, w2_ap, dff, acc, acckey, ws13, ws2, hbuf, sbuf, first_write):
    S, ps = K.S, K.ps
    nun = dff // 256
    assert nun % 2 == 0
    it = 0

    def load(g):
        return (ws13.load(K.wunit(w1_ap, g * 256, 256)), ws13.load(K.wunit(w3_ap, g * 256, 256)),
                ws2.load(w2_ap[g * 256:(g + 1) * 256, :].rearrange("(m p) n -> p m n", p=128)))
    L = {0: load(0)}
    for g in range(nun):
        (s1, k1), (s3, k3), _ = L[g]
        pr = g // 2
        hb, hk = hbuf[pr % 2], 'hb%d' % (pr % 2)
        for m in range(2):
            mm_ = (g % 2) * 2 + m
            for (c0, n) in halves:
                b = it % 2
                it += 1
                K.mm_group(ps[b][:, 0:n], [s1[:, k, m * 128:(m + 1) * 128] for k in range(NCH)],
                           [uT[:, k, c0:c0 + n] for k in range(NCH)], r=[k1, ukey], w=['ps%d' % b])
                K.mm_group(ps[2 + b][:, 0:n], [s3[:, k, m * 128:(m + 1) * 128] for k in range(NCH)],
                           [uT[:, k, c0:c0 + n] for k in range(NCH)], r=[k3, ukey], w=['ps%d' % (2 + b)])
                sb_, sk = sbuf[b], 'sb%d' % b
                S.op('act', lambda e, b=b, n=n, sb_=sb_: e.activation(sb_[:, 0:n], ps[b][:, 0:n], AF.Silu),
                     r=['ps%d' % b], w=[sk])
                S.op('dve', lambda e, b=b, n=n, sb_=sb_, hb=hb, mm_=mm_, c0=c0: e.tensor_tensor(
                    hb[:, mm_, c0:c0 + n], ps[2 + b][:, 0:n], sb_[:, 0:n], ALU.mult), r=['ps%d' % (2 + b), sk], w=[hk])
        if g + 1 < nun:
            L[g + 1] = load(g + 1)
        if g % 2 == 0:
            continue
        (sa, ka), (sb2, kb2) = L[g - 1][2], L[g][2]
        for d in range(NCH):
            for (c0, n) in halves:
                b = it % 2
                it += 1
                K.mm_group(ps[4 + b][:, 0:n],
                           [sa[:, m, d * 128:(d + 1) * 128] for m in range(2)] + [sb2[:, m, d * 128:(d + 1) * 128] for m in range(2)],
                           [hb[:, m, c0:c0 + n] for m in range(4)], r=[ka, kb2, hk], w=['ps%d' % (4 + b)])
                ak = '%s_%d' % (acckey, d)
                if pr == 0 and first_write:
                    S.op('act', lambda e, b=b, n=n, d=d, c0=c0: e.activation(acc[:, d, c0:c0 + n], ps[4 + b][:, 0:n], AF.Copy),
                         r=['ps%d' % (4 + b)], w=[ak])
                else:
                    S.op('dve', lambda e, b=b, n=n, d=d, c0=c0: e.tensor_tensor(
                        acc[:, d, c0:c0 + n], acc[:, d, c0:c0 + n], ps[4 + b][:, 0:n], ALU.add),
                        r=['ps%d' % (4 + b), ak], w=[ak])
        del L[g - 1]


def phase_ffn0(K, t):
    S, A, I = K.S, K.A, K.I
    col0 = t * 512
    cond = 0 if t == 0 else 1
    xt = A.alloc((NCH, 512))
    uT = A.alloc((NCH, 512), BF16)
    acc = A.alloc((NCH, 512))
    hbuf = [A.alloc((4, 512), BF16) for _ in range(2)]
    sbuf = [A.alloc(512) for _ in range(2)]
    ws13 = K.WS('wf13', 4, (NCH, 256))
    ws2 = K.WS('wf2', 3, (2, D))
    load_x_tile(K, xt, K.XS1, col0, 512, 'xt', 'ld_x_f0')
    for c in range(NCH):
        S.op('dve', lambda e, c=c: e.tensor_scalar(uT[:, c, :], xt[:, c, :], K.modc(0, 4, c, cond), K.modc(0, 3, c, cond),
                                                   ALU.mult, ALU.add), r=['xt', 'mod'], w=['uT'])
    ffn_pass(K, uT, 'uT', [(0, 512)], I['l0_ffn_w1'], I['l0_ffn_w3'], I['l0_ffn_w2'], DFF, acc, 'acc', ws13, ws2, hbuf, sbuf, True)
    post_norm(K, 0, 5, 1, xt, 'xt', lambda m: (acc[:, m, :], 'acc_%d' % m), [(0, 512, cond)], 512, A)
    S.op('sp', lambda e: [e.dma_start(out=K.XS2[:, :, col0:col0 + 512], in_=xt)], r=['xt'], chan='st_x_f0')


def finalize(K):
    nc, S = K.nc, K.S
    S.finish()
    names = [('e', e) for e in Sched.ENG] + [('c', c) for c in S.chan_cnt]
    st = K.st
    sems = {k: st.enter_context(nc.semaphore(("s_%s_%s" % k).replace('#', '_'))) for k in names}
    block = st.enter_context(nc.Block())

    @block.tensor
    def _(e):
        S.emit('pe', e, sems)

    @block.scalar
    def _(e):
        S.emit('act', e, sems)

    @block.vector
    def _(e):
        S.emit('dve', e, sems)

    @block.gpsimd
    def _(e):
        S.emit('pool', e, sems)

    @block.sync
    def _(e):
        S.emit('sp', e, sems)
    st.close()
    return nc


STAGE = 99


def build_all(debug=False, stage=None):
    stage = STAGE if stage is None else stage
    K = build_program(debug)

    def sep():
        K.S.barrier()
        K.A.reset()
    if stage >= 1:
        phase_mixer0(K, 'P')
        sep()
    if stage >= 2:
        phase_mixer0(K, 'S')
        sep()
    if stage >= 3:
        for t in range(3):
            phase_ffn0(K, t)
            sep()
    if stage >= 4:
        phase_filter(K, 256)
        sep()
        phase_filter(K, 1024)
        sep()
    if stage >= 5:
        phase_hyena(K, 'P')
        sep()
        phase_hyena(K, 'S')
        sep()
    if stage >= 6:
        phase_moe(K)
        sep()
        phase_final(K)
        sep()
    return finalize(K)


def _dft_consts(L):
    t = np.arange(L, dtype=np.float64)
    th = np.pi * np.outer(t, t) / L
    sgn = np.where(np.arange(L) % 2 == 0, 1.0, -1.0)
    FC = np.cos(th)
    FS = -np.sin(th)
    FS[:, 0] = sgn
    IC = np.cos(th) / L
    IC[0, :] = 1.0 / (2 * L)
    IS = -np.sin(th) / L
    IS[0, :] = sgn / (2 * L)
    NY2 = np.zeros((L, L))
    NY2[:, 0] = 2 * sgn
    return np.stack([FC, FS, IC, IS, NY2]).astype(np.float32)


def _feat(L):
    t = np.arange(L, dtype=np.float32)
    t01 = t / np.float32(L - 1)
    w = np.float32(2.0 * math.pi) * t / np.float32(L)
    f = np.linspace(1e-4, 15, 16, dtype=np.float32)
    fw = w[:, None] * f[None, :]
    feat = np.concatenate([t01[:, None], np.cos(fw), -np.sin(fw)], -1).astype(np.float32)
    return np.ascontiguousarray(feat.T)


def _cols(v, n):
    return np.ascontiguousarray(np.asarray(v, np.float32).reshape(n, 128).T)


def make_in_maps(inp):
    f = lambda a: np.ascontiguousarray(np.asarray(a, np.float32))
    shared = {}
    def tile_w(W, ncols):
        W = np.asarray(W, np.float32)
        N = W.shape[1]
        return np.ascontiguousarray(W.reshape(16, 128, N // ncols, ncols).transpose(2, 1, 0, 3))
    for nm, nc_ in [('l0_ada_w', 512), ('l0_w_in', 512), ('l0_w_out', 512), ('l0_ffn_w1', 256), ('l0_ffn_w3', 256),
                    ('l1_ada_w', 512), ('l1_w_in', 128), ('l1_w_out', 512)]:
        shared[nm] = tile_w(inp[nm], nc_)
    shared['l0_ffn_w2'] = f(inp['l0_ffn_w2'])
    shared['exp_w1'] = np.stack([tile_w(inp['l1_exp_w1'][e_], 256) for e_ in range(NEXP)])
    shared['exp_w3'] = np.stack([tile_w(inp['l1_exp_w3'][e_], 256) for e_ in range(NEXP)])
    shared['exp_w2'] = f(inp['l1_exp_w2'])
    shared['lru_wr'] = f(np.transpose(inp['l0_lru_w_r'], (2, 0, 1, 3)))
    shared['lru_wi'] = f(np.transpose(inp['l0_lru_w_i'], (2, 0, 1, 3)))
    shared['router'] = f(np.asarray(inp['l1_router']).reshape(16, 128, 8).transpose(1, 0, 2))
    shared['qg'] = f(np.broadcast_to(np.asarray(inp['l0_q_norm'])[None, :], (128, 128)))
    shared['kg'] = f(np.broadcast_to(np.asarray(inp['l0_k_norm'])[None, :], (128, 128)))
    tt = np.arange(1024)
    inv = 1.0 / (10000.0 ** (np.arange(32, dtype=np.float32) / 32))
    ang = np.concatenate([(tt // 64).astype(np.float32)[:, None] * inv, (tt % 64).astype(np.float32)[:, None] * inv], -1)
    rope = np.stack([np.cos(ang), np.sin(ang)], 1).astype(np.float32)
    shared['rope'] = f(rope.reshape(8, 128, 2, 64).transpose(1, 0, 2, 3))
    shared['featT256'] = _feat(256)
    shared['featT1024'] = _feat(1024)
    shared['fw1'] = f(inp['l1_filt_w1'])
    shared['fw2'] = f(inp['l1_filt_w2'])
    shared['fw3'] = f(inp['l1_filt_w3'])
    shared['pk64'] = f(np.stack([inp['l1_filt_b1'], inp['l1_filt_f1'], inp['l1_filt_b2'], inp['l1_filt_f2']], 1))
    shared['decay'] = f(np.asarray(inp['l1_filt_decay']).reshape(1, 8192))
    for L in (256, 1024):
        shared['nt01_%d' % L] = f((-(np.arange(L, dtype=np.float32) / np.float32(L - 1))).reshape(L // 128, 128).T)
        shared['dft%d' % L] = _dft_consts(L)
    shared['ident'] = np.eye(128, dtype=np.float32)
    sel = np.zeros((8, 8, 128), np.float32)
    for e_ in range(8):
        sel[e_, e_, :] = 1.0
    shared['sel'] = sel.reshape(8, 1024)
    shared['tri'] = np.triu(np.ones((128, 128), np.float32), 1)
    shared['iota'] = np.ascontiguousarray(np.broadcast_to(np.arange(512, dtype=np.float32)[None, :], (128, 512)))
    pk_common = np.zeros((128, NPK), np.float32)

    def put(name, arr):
        o, w = PK[name]
        assert arr.shape == (128, w), (name, arr.shape)
        pk_common[:, o:o + w] = arr
    put('ada_b0', _cols(inp['l0_ada_b'], 96))
    put('ada_b1', _cols(inp['l1_ada_b'], 96))
    put('ln', np.concatenate([_cols(inp[n], 16) for n in ['l0_ln1_g', 'l0_ln1_b', 'l0_ln2_g', 'l0_ln2_b',
                                                         'l1_ln1_g', 'l1_ln1_b', 'l1_ln2_g', 'l1_ln2_b']], 1))
    put('conv_w', f(np.asarray(inp['l0_lru_conv_w']).reshape(4, 8, 128).transpose(2, 1, 0).reshape(128, 32)))
    put('conv_b', _cols(inp['l0_lru_conv_b'], 8))
    put('lam', f(np.asarray(inp['l0_lru_lambda']).reshape(2, 8, 128).transpose(2, 0, 1).reshape(128, 16)))
    put('b_r', f(np.asarray(inp['l0_lru_b_r']).reshape(2, 8, 128).transpose(2, 0, 1).reshape(128, 16)))
    put('b_i', f(np.asarray(inp['l0_lru_b_i']).reshape(2, 8, 128).transpose(2, 0, 1).reshape(128, 16)))
    put('short_w', f(np.asarray(inp['l1_short_w']).reshape(3, 48, 128).transpose(2, 1, 0).reshape(128, 144)))
    put('short_b', _cols(inp['l1_short_b'], 48))
    put('fbias', f(np.asarray(inp['l1_filt_bias']).reshape(2, 16, 128).transpose(2, 0, 1).reshape(128, 32)))
    xp = np.asarray(inp['x_prompt'], np.float32)
    xs = np.asarray(inp['x_sample'], np.float32)
    maps = []
    for i in range(8):
        s, q = i // 4, i % 4
        m = dict(shared)
        X = np.concatenate([xp[2 * i], xp[2 * i + 1], xs[s]], 0)
        m['xT'] = f(X.T.reshape(16, 128, NCOL).transpose(1, 0, 2))
        cond = np.stack([np.asarray(inp['c_ctx'], np.float32), np.asarray(inp['c'], np.float32)[s]], 0)
        m['condT'] = f(cond.T.reshape(16, 128, 2).transpose(1, 0, 2))
        pkk = pk_common.copy()
        o, w = PK['h0']
        pkk[:, o:o + w] = np.asarray(inp['state_l0_lru'], np.float32)[s].reshape(2, 8, 128).transpose(2, 0, 1).reshape(128, 16)
        o, w = PK['qmask']
        pkk[:, o:o + w] = 0.0
        pkk[:, o + q] = 1.0
        m['pk'] = pkk
        m['ckT'] = f(np.asarray(inp['cache_l0_k'], np.float32)[s].transpose(2, 1, 0))
        m['cv'] = f(np.asarray(inp['cache_l0_v'], np.float32)[s].reshape(2, 128, 256).transpose(1, 0, 2))
        maps.append(m)
    return maps


_PROG = {}


def kernel(**inputs):
    maps = make_in_maps(inputs)
    if 'nc' not in _PROG:
        _PROG['nc'] = build_all()
    res = run_bass_kernel_spmd(_PROG['nc'], maps, core_ids=list(range(8)))
    y_prompt = np.zeros((16, 256, D), np.float32)
    y_sample = np.zeros((2, 1024, D), np.float32)
    nk = np.zeros((16, 256, 2, 128), np.float32)
    nv = np.zeros((16, 256, 2, 128), np.float32)
    nh = np.zeros((16, 2, 1024), np.float32)
    for i in range(8):
        r = res.results[i]
        yT = np.asarray(r['yT'])
        Y = yT.transpose(2, 1, 0).reshape(NMOE, D)
        y_prompt[2 * i] = Y[0:256]
        y_prompt[2 * i + 1] = Y[256:512]
        s, q = i // 4, i % 4
        y_sample[s, q * 256:(q + 1) * 256] = Y[512:768]
        ko = np.asarray(r['kout']).reshape(2, 256, 2, 128)
        vo = np.asarray(r['vout']).reshape(2, 256, 2, 128)
        nk[2 * i], nk[2 * i + 1] = ko[0], ko[1]
        nv[2 * i], nv[2 * i + 1] = vo[0], vo[1]
        ho = np.asarray(r['hout']).reshape(128, 2, 2, 8)
        hh = ho.transpose(1, 2, 3, 0).reshape(2, 2, 1024)
        nh[2 * i], nh[2 * i + 1] = hh[0], hh[1]
    return (y_prompt, y_sample, nk, nv, nh)


def phase_filter(K, L):
    S, A, I, ps = K.S, K.A, K.I, K.ps
    nt = L // 128
    KFL = K.KF[L]
    featb = A.alloc(L, BF16)
    fw1b = A.alloc(64, BF16)
    fw2b = A.alloc(64, BF16)
    fw3b = A.alloc(8192, BF16)
    pk64 = A.alloc(4)
    absd = A.alloc(8192)
    nt01 = A.alloc(nt)
    FCb = A.alloc((nt, L), BF16)
    FSb = A.alloc((nt, L), BF16)
    NYb = A.alloc((nt, 128), BF16)
    arg = A.alloc(512)
    rr = A.alloc(512)
    h1b = A.alloc(L, BF16)
    h2b = A.alloc(L, BF16)
    E = A.alloc((2, 512))
    hdec = A.alloc((2, 512))
    hs = A.alloc((nt, 512), BF16)
    hd = A.alloc((nt, 512), BF16)
    h1p = A.alloc((nt, 512), BF16)
    stg = [A.alloc(512) for _ in range(6)]
    dft = I['dft%d' % L]
    S.op('pool', lambda e: [e.dma_start(out=featb[0:33, :], in_=I['featT%d' % L])], w=['featb'], chan='f_feat')
    S.op('pool', lambda e: [e.dma_start(out=fw1b[0:33, :], in_=I['fw1'])], w=['fw1b'], chan='f_w1')
    S.op('pool', lambda e: [e.dma_start(out=fw2b[0:64, :], in_=I['fw2'])], w=['fw2b'], chan='f_w2')
    S.op('pool', lambda e: [e.dma_start(out=fw3b[0:64, :], in_=I['fw3'], max_dma_last_dim=8192)], w=['fw3b'], chan='f_w3')
    S.op('sp', lambda e: [e.dma_start(out=pk64[0:64, :], in_=I['pk64'])], w=['pk64'], chan='f_pk64')
    S.op('sp', lambda e: [e.dma_start(out=absd, in_=I['decay'].partition_broadcast(128))], w=['absd'], chan='f_dec')
    S.op('sp', lambda e: [e.dma_start(out=nt01, in_=I['nt01_%d' % L])], w=['nt01'], chan='f_nt01')
    S.op('pool', lambda e: [e.dma_start(out=FCb, in_=dft[0].rearrange("(a p) f -> p a f", p=128))], w=['FCb'], chan='f_fc')
    S.op('pool', lambda e: [e.dma_start(out=FSb, in_=dft[1].rearrange("(a p) f -> p a f", p=128))], w=['FSb'], chan='f_fs')
    S.op('pool', lambda e: [e.dma_start(out=NYb, in_=dft[4][:, 0:128].rearrange("(a p) f -> p a f", p=128))], w=['NYb'], chan='f_ny')
    S.op('act', lambda e: e.activation(absd, absd, AF.Abs), r=['absd'], w=['absd'])

    def sin_layer(lhsT, lkey, kp, rhsb, rkey, bcol, fcol, outb, okey):
        for b in range(L // min(L, 512)):
            n = min(L, 512)
            cs = slice(b * n, (b + 1) * n)
            S.op('pe', lambda e, cs=cs, n=n: e.matmul(ps[0][0:64, 0:n], lhsT[0:kp, 0:64], rhsb[0:kp, cs], start=True, stop=True),
                 r=[lkey, rkey], w=['ps0'])
            S.op('dve', lambda e, n=n: e.tensor_scalar(arg[0:64, 0:n], ps[0][0:64, 0:n], pk64[0:64, bcol:bcol + 1],
                                                       pk64[0:64, fcol:fcol + 1], ALU.add, ALU.mult), r=['ps0', 'pk64'], w=['arg'])
            S.op('dve', lambda e, n=n: e.tensor_scalar(rr[0:64, 0:n], arg[0:64, 0:n], 1.0 / (2 * math.pi), MAGIC, ALU.mult, ALU.add),
                 r=['arg'], w=['rr'])
            S.op('dve', lambda e, n=n: e.tensor_scalar(rr[0:64, 0:n], rr[0:64, 0:n], MAGIC, -2 * math.pi, ALU.subtract, ALU.mult),
                 r=['rr'], w=['rr'])
            S.op('dve', lambda e, n=n: e.tensor_tensor(arg[0:64, 0:n], arg[0:64, 0:n], rr[0:64, 0:n], ALU.add), r=['arg', 'rr'], w=['arg'])
            S.op('dve', lambda e, n=n: e.tensor_scalar(arg[0:64, 0:n], arg[0:64, 0:n], 3.1415925, -3.1415925, ALU.min, ALU.max), r=['arg'], w=['arg'])
            S.op('act', lambda e, cs=cs, n=n: e.activation(outb[0:64, cs], arg[0:64, 0:n], AF.Sin), r=['arg'], w=[okey])

    sin_layer(fw1b, 'fw1b', 33, featb, 'featb', 0, 1, h1b, 'h1b')
    sin_layer(fw2b, 'fw2b', 64, h1b, 'h1b', 2, 3, h2b, 'h2b')
    si = 0
    for cu in range(8):
        order, chq = cu // 4, cu % 4
        cbase = order * 2048 + chq * 512
        for tb in range(nt):
            for side in range(2):
                cc = side * 4096 + cbase
                S.op('pe', lambda e, side=side, tb=tb, cc=cc: e.matmul(ps[side], h2b[0:64, tb * 128:(tb + 1) * 128],
                                                                      fw3b[0:64, cc:cc + 512], start=True, stop=True),
                     r=['h2b', 'fw3b'], w=['ps%d' % side])
                S.op('act', lambda e, side=side, tb=tb, cc=cc: e.activation(E[:, side, :], absd[:, cc:cc + 512], AF.Exp,
                                                                            scale=nt01[:, tb:tb + 1]), r=['absd', 'nt01'], w=['E%d' % side])
                S.op('dve', lambda e, side=side: e.tensor_tensor(hdec[:, side, :], ps[side], E[:, side, :], ALU.mult),
                     r=['ps%d' % side, 'E%d' % side], w=['hdec'])
            if tb == 0:
                S.op('dve', lambda e: e.memset(hdec[0:1, 1, :], 0.0), r=['hdec'], w=['hdec'])
            S.op('dve', lambda e, tb=tb: e.tensor_tensor(hs[:, tb, :], hdec[:, 0, :], hdec[:, 1, :], ALU.add), r=['hdec'], w=['hs'])
            S.op('dve', lambda e, tb=tb: e.tensor_tensor(hd[:, tb, :], hdec[:, 0, :], hdec[:, 1, :], ALU.subtract), r=['hdec'], w=['hd'])
            S.op('act', lambda e, tb=tb: e.activation(h1p[:, tb, :], hdec[:, 1, :], AF.Copy), r=['hdec'], w=['h1p'])
        for fc in range(nt):
            fs_ = slice(fc * 128, (fc + 1) * 128)
            K.mm_group(ps[2], [FCb[:, tb, fs_] for tb in range(nt)], [hs[:, tb, :] for tb in range(nt)], r=['FCb', 'hs'], w=['ps2'])
            lh = [FSb[:, tb, fs_] for tb in range(nt)]
            rh = [hd[:, tb, :] for tb in range(nt)]
            if fc == 0:
                lh += [NYb[:, tb, :] for tb in range(nt)]
                rh += [h1p[:, tb, :] for tb in range(nt)]
            K.mm_group(ps[3], lh, rh, r=['FSb', 'NYb', 'hd', 'h1p'], w=['ps3'])
            a_, b_, d_ = stg[si % 2 * 3], stg[si % 2 * 3 + 1], stg[si % 2 * 3 + 2]
            ka, kb, kd = 'stgA%d' % (si % 2), 'stgB%d' % (si % 2), 'stgD%d' % (si % 2)
            si += 1
            S.op('act', lambda e, a_=a_: e.activation(a_, ps[2], AF.Copy), r=['ps2'], w=[ka])
            S.op('dve', lambda e, b_=b_: e.tensor_copy(b_, ps[3]), r=['ps3'], w=[kb])
            dsl = slice(cbase, cbase + 512)
            S.op('sp', lambda e, a_=a_, fc=fc, dsl=dsl: [e.dma_start(out=KFL[0, fc, :, dsl], in_=a_)], r=[ka], w=['KF'], chan=ka)
            if fc == 0:
                S.op('act', lambda e, a_=a_, d_=d_: e.activation(d_, a_, AF.Copy), r=[ka], w=[kd])
                S.op('act', lambda e, b_=b_, d_=d_: e.activation(d_[0:1, :], b_[0:1, :], AF.Copy), r=[kb, kd], w=[kd])
                S.op('dve', lambda e, b_=b_: e.memset(b_[0:1, :], 0.0), r=[kb, kd], w=[kb])
                S.op('sp', lambda e, d_=d_, dsl=dsl: [e.dma_start(out=KFL[2, 0, :, dsl], in_=d_)], r=[kd], w=['KF'], chan=kd)
            S.op('sp', lambda e, b_=b_, fc=fc, dsl=dsl: [e.dma_start(out=KFL[1, fc, :, dsl], in_=b_)], r=[kb], w=['KF'], chan=kb)


def phase_hyena(K, grp):
    S, A, I, ps, psb = K.S, K.A, K.I, K.ps, K.psb
    if grp == 'P':
        col0, ncols, nseg, L, cond = 0, 512, 2, 256, 0
    else:
        col0, ncols, nseg, L, cond = 512, 1024, 1, 1024, 1
    nt = L // 128
    nb = ncols // 512
    KFL = K.KF[L]
    dft = I['dft%d' % L]
    uT = A.alloc((NCH, ncols), BF16)
    base = A.off
    xtmp = A.alloc((NCH, 512))
    for t in range(nb):
        load_x_tile(K, xtmp, K.XS2, col0 + t * 512, 512, 'xtmp', 'ld_xtmp_h')
        for c in range(NCH):
            S.op('dve', lambda e, c=c, t=t: e.tensor_scalar(
                uT[:, c, t * 512:(t + 1) * 512], xtmp[:, c, :], K.modc(1, 1, c, cond), K.modc(1, 0, c, cond),
                ALU.mult, ALU.add), r=['xtmp', 'mod'], w=['uT'])
    S.barrier()
    A.off = base
    M = [A.alloc((nt, L), BF16) for _ in range(4)]
    for i in range(4):
        S.op('pool', lambda e, i=i: [e.dma_start(out=M[i], in_=dft[i].rearrange("(a p) f -> p a f", p=128))],
             w=['M%d' % i], chan='h_m%d' % i)
    ws = K.WS('wh', 3, (NCH, 128))
    pbs = [A.alloc((nseg, L + 2)) for _ in range(3)]
    cx = [A.alloc(ncols) for _ in range(3)]
    zcur = A.alloc(ncols)
    srcb = A.alloc(ncols, BF16)
    zt = A.alloc((nt, 128), BF16)
    KA = [A.alloc((nt, 128)) for _ in range(2)]
    KB = [A.alloc((nt, 128)) for _ in range(2)]
    KD = [A.alloc((nt, 128)) for _ in range(2)]
    Pre = A.alloc((nt, 128), BF16)
    Pim = A.alloc((nt, 128), BF16)
    t1 = A.alloc(512)
    t2 = A.alloc(512)
    tmp = A.alloc(ncols)
    for i in range(3):
        S.op('dve', lambda e, i=i: e.memset(pbs[i], 0.0), w=['pb%d' % i])
    swo, sbo, fbo = PK['short_w'][0], PK['short_b'][0], PK['fbias'][0]
    seg3 = lambda ap: ap.rearrange("p (s l) -> p s l", l=L)
    NQ = min(L, 512)
    kit = 0
    for c in range(NCH):
        for wi_ in range(3):
            slot, wkey = ws.load(K.wunit(I['l1_w_in'], wi_ * 2048 + c * 128, 128))
            eidx = wi_ * 16 + c
            for b in range(nb):
                K.mm_group(ps[b], [slot[:, k, :] for k in range(NCH)], [uT[:, k, b * 512:(b + 1) * 512] for k in range(NCH)],
                           r=[wkey, 'uT'], w=['ps%d' % b])
                if grp == 'P':
                    S.op('act', lambda e, b=b, wi_=wi_: e.activation(pbs[wi_][:, :, 1:L + 1], seg3(ps[b]), AF.Copy),
                         r=['ps%d' % b], w=['pb%d' % wi_])
                else:
                    S.op('act', lambda e, b=b, wi_=wi_: e.activation(pbs[wi_][:, 0, 1 + b * 512:1 + (b + 1) * 512], ps[b], AF.Copy),
                         r=['ps%d' % b], w=['pb%d' % wi_])
            o3 = seg3(cx[wi_])
            S.op('dve', lambda e, wi_=wi_, eidx=eidx, o3=o3: e.tensor_scalar(
                o3, pbs[wi_][:, :, 0:L], K.pk[:, swo + eidx * 3:swo + eidx * 3 + 1], K.pk[:, sbo + eidx:sbo + eidx + 1],
                ALU.mult, ALU.add), r=['pb%d' % wi_, 'pk'], w=['cx%d' % wi_])
            for k in (1, 2):
                S.op('dve', lambda e, wi_=wi_, eidx=eidx, o3=o3, k=k: e.scalar_tensor_tensor(
                    o3, pbs[wi_][:, :, k:k + L], K.pk[:, swo + eidx * 3 + k:swo + eidx * 3 + k + 1], o3, ALU.mult, ALU.add),
                    r=['pb%d' % wi_, 'pk', 'cx%d' % wi_], w=['cx%d' % wi_])
        for n in range(2):
            src, skey = (cx[2], 'cx2') if n == 0 else (zcur, 'zcur')
            gate, gkey = (cx[0], 'cx0') if n == 0 else (cx[1], 'cx1')
            kk = 0
            csl = slice(n * 2048 + c * 128, n * 2048 + (c + 1) * 128)
            S.op('sp', lambda e, kk=kk, csl=csl: [e.dma_start(out=KA[kk], in_=KFL[0, :, :, csl].rearrange("a p f -> p a f"))],
                 r=['KF'], w=['KA%d' % kk], chan='h_ka%d' % kk)
            S.op('sp', lambda e, kk=kk, csl=csl: [e.dma_start(out=KB[kk], in_=KFL[1, 0:nt, :, csl].rearrange("a p f -> p a f"))],
                 r=['KF'], w=['KB%d' % kk], chan='h_kb%d' % kk)
            S.op('sp', lambda e, kk=kk, csl=csl: [e.dma_start(out=KD[kk][:, 1:nt, :], in_=KFL[0, 1:nt, :, csl].rearrange("a p f -> p a f"))],
                 r=['KF'], w=['KD%d' % kk], chan='h_kd%d' % kk)
            S.op('sp', lambda e, kk=kk, csl=csl: [e.dma_start(out=KD[kk][:, 0, :], in_=KFL[2, 0, :, csl])],
                 r=['KF'], w=['KD%d' % kk], chan='h_kd%d' % kk)
            S.op('act', lambda e, src=src: e.activation(srcb, src, AF.Copy), r=[skey], w=['srcb'])
            for seg in range(nseg):
                def fn(e, seg=seg):
                    ins = None
                    for tb in range(nt):
                        ins = e.transpose(psb[6][:, tb * 128:(tb + 1) * 128], srcb[:, seg * L + tb * 128:seg * L + (tb + 1) * 128], K.ident)
                    return ins
                S.op('pe', fn, r=['srcb', 'ident'], w=['ps6'])
                S.op('act', lambda e: e.activation(zt, psb[6][:, 0:nt * 128].rearrange("p (a t) -> p a t", t=128), AF.Copy),
                     r=['ps6'], w=['zt'])
                for fc in range(nt):
                    bq, off = fc // 4, (fc % 4) * 128
                    fs_ = slice(fc * 128, (fc + 1) * 128)
                    K.mm_group(ps[0 + bq][:, off:off + 128], [M[0][:, tb, fs_] for tb in range(nt)], [zt[:, tb, :] for tb in range(nt)],
                               r=['M0', 'zt'], w=['ps%d' % bq])
                    K.mm_group(ps[2 + bq][:, off:off + 128], [M[1][:, tb, fs_] for tb in range(nt)], [zt[:, tb, :] for tb in range(nt)],
                               r=['M1', 'zt'], w=['ps%d' % (2 + bq)])
                for bq in range((nt + 3) // 4):
                    nf = min(4, nt - bq * 4)
                    w_ = nf * 128
                    fsl = slice(bq * 4, bq * 4 + nf)
                    f2 = lambda ap, fsl=fsl: ap[:, fsl, :]
                    v3 = lambda ap, w_=w_: ap[:, 0:w_].rearrange("p (a f) -> p a f", f=128)
                    ur, ui = ps[bq], ps[2 + bq]
                    ukr, uki = 'ps%d' % bq, 'ps%d' % (2 + bq)
                    S.op('dve', lambda e, ur=ur, f2=f2, v3=v3, kk=kk: e.tensor_tensor(v3(t1), v3(ur), f2(KA[kk]), ALU.mult), r=[ukr, 'KA%d' % kk], w=['t1'])
                    S.op('dve', lambda e, ui=ui, f2=f2, v3=v3, kk=kk: e.tensor_tensor(v3(t2), v3(ui), f2(KB[kk]), ALU.mult), r=[uki, 'KB%d' % kk], w=['t2'])
                    S.op('dve', lambda e, f2=f2, v3=v3: e.tensor_tensor(f2(Pre), v3(t1), v3(t2), ALU.subtract), r=['t1', 't2'], w=['Pre'])
                    S.op('dve', lambda e, ur=ur, f2=f2, v3=v3, kk=kk: e.tensor_tensor(v3(t1), v3(ur), f2(KB[kk]), ALU.mult), r=[ukr, 'KB%d' % kk, 'Pre'], w=['t1'])
                    S.op('dve', lambda e, ui=ui, f2=f2, v3=v3, kk=kk: e.tensor_tensor(v3(t2), v3(ui), f2(KD[kk]), ALU.mult), r=[uki, 'KD%d' % kk, 'Pre'], w=['t2'])
                    S.op('dve', lambda e, f2=f2, v3=v3: e.tensor_tensor(f2(Pim), v3(t1), v3(t2), ALU.add), r=['t1', 't2'], w=['Pim'])
                for hq in range(L // NQ):
                    tsl = slice(hq * NQ, (hq + 1) * NQ)
                    yb_ = ps[4 + hq % 2]
                    yk = 'ps%d' % (4 + hq % 2)
                    K.mm_group(yb_[:, 0:NQ], [Pre[:, fc, :] for fc in range(nt)] + [Pim[:, fc, :] for fc in range(nt)],
                               [M[2][:, fc, tsl] for fc in range(nt)] + [M[3][:, fc, tsl] for fc in range(nt)],
                               r=['Pre', 'Pim', 'M2', 'M3'], w=[yk])
                    gs = slice(seg * L + hq * NQ, seg * L + (hq + 1) * NQ)
                    S.op('dve', lambda e, gs=gs, yb_=yb_, src=src, n=n, c=c: e.scalar_tensor_tensor(
                        tmp[:, gs], src[:, gs], K.pk[:, fbo + n * 16 + c:fbo + n * 16 + c + 1], yb_[:, 0:NQ], ALU.mult, ALU.add),
                        r=[yk, skey, 'pk'], w=['tmp'])
            S.op('dve', lambda e, gate=gate: e.tensor_tensor(zcur, tmp, gate, ALU.mult), r=['tmp', gkey, 'srcb', skey], w=['zcur'])
        S.op('sp', lambda e, c=c: [e.dma_start(out=K.ZZ[:, c, col0:col0 + ncols], in_=zcur)], r=['zcur'], w=['ZZ'], chan='st_zz')
    S.barrier()
    A.off = base
    zzT = A.alloc((NCH, 512), BF16)
    ws2 = K.WS('who', 2, (NCH, 512))
    mark = A.off
    for t in range(nb):
        A.off = mark
        S.op('pool', lambda e, t=t: [e.dma_start(out=zzT, in_=K.ZZ[:, :, col0 + t * 512:col0 + (t + 1) * 512])],
             r=['ZZ'], w=['zzT'], chan='ld_zz')
        wout_postnorm(K, 1, I['l1_w_out'], zzT, 'zzT', 0, 512, [(0, 512, cond)], _ColShift(K.XS2, col0 + t * 512),
                      _ColShift(K.XS3, col0 + t * 512), ws2, 2, 2, 'h1')


def load_piece(K, dst, src, piece, tmpq, key):
    S = K.S
    if piece < 2:
        S.op('sp', lambda e: [e.dma_start(out=dst, in_=src[:, :, piece * 256:(piece + 1) * 256])], w=[key], chan='ld_pc')
        return
    qo = PK['qmask'][0]
    for r in range(4):
        S.op('sp', lambda e, r=r: [e.dma_start(out=tmpq, in_=src[:, :, 512 + r * 256:512 + (r + 1) * 256])], w=['tmpq'], chan='ld_pq')
        if r == 0:
            S.op('dve', lambda e, r=r: e.tensor_scalar(dst, tmpq, K.pk[:, qo + r:qo + r + 1], None, ALU.mult), r=['tmpq', 'pk'], w=[key])
        else:
            S.op('dve', lambda e, r=r: e.scalar_tensor_tensor(dst, tmpq, K.pk[:, qo + r:qo + r + 1], dst, ALU.mult, ALU.add),
                 r=['tmpq', 'pk', key], w=[key])


def phase_final(K):
    S, A, O = K.S, K.A, K.O
    xq = A.alloc((NCH, 256))
    tmpq = A.alloc((NCH, 256))
    mo = A.alloc((NCH, 256))
    mark = A.off
    for piece in range(3):
        A.off = mark
        cond = 0 if piece < 2 else 1
        cs = slice(piece * 256, (piece + 1) * 256)
        load_piece(K, xq, K.XS3, piece, tmpq, 'xt')
        S.op('sp', lambda e, cs=cs: [e.dma_start(out=mo, in_=K.MO[:, :, cs])], r=['MO'], w=['mo'], chan='ld_mo')
        post_norm(K, 1, 5, 3, xq, 'xt', lambda m: (mo[:, m, :], 'mo'), [(0, 256, cond)], 256, A)
        S.op('sp', lambda e, cs=cs: [e.dma_start(out=O['yT'][:, :, cs], in_=xq)], r=['xt'], chan='st_y')


I32 = mybir.dt.int32
CAP = 256
MOE_THR = (257, 513)


def phase_moe(K):
    S, A, I, ps, psb = K.S, K.A, K.I, K.ps, K.psb
    NTB = NMOE // 128
    u_tok = A.alloc((NTB, D), BF16)
    maskf = A.alloc((NTB, 8))
    combf = A.alloc((NTB, 8))
    posf = [A.alloc((NTB, 8)) for _ in range(2)]
    cntf = A.alloc(8)
    cnt_i = A.alloc(8).bitcast(I32)
    iota = A.alloc(512)
    tri = A.alloc(128, BF16)
    base = A.off
    uT = A.alloc((NCH, NMOE), BF16)
    loT = A.alloc((NCH, NMOE), BF16)
    xq = A.alloc((NCH, 256))
    tmpq = A.alloc((NCH, 256))
    rf = A.alloc((NCH, 8))
    rhi = A.alloc((NCH, 8), BF16)
    rlo = A.alloc((NCH, 8), BF16)
    maskb = A.alloc((NTB, 8), BF16)
    ut = A.alloc(256)
    lg = A.alloc(8)
    m8 = A.alloc(8)
    sm = A.alloc(8)
    c2 = A.alloc(8)
    S.op('sp', lambda e: [e.dma_start(out=rf, in_=I['router'])], w=['rf'], chan='ld_rt')
    S.op('sp', lambda e: [e.dma_start(out=iota, in_=I['iota'])], w=['iota'], chan='ld_iota')
    S.op('pool', lambda e: [e.dma_start(out=tri, in_=I['tri'])], w=['tri'], chan='ld_tri')
    S.op('dve', lambda e: e.tensor_copy(rhi, rf), r=['rf'], w=['rhi'])
    S.op('dve', lambda e: e.tensor_tensor(rlo, rf, rhi, ALU.subtract), r=['rf', 'rhi'], w=['rlo'])
    for piece in range(3):
        cond = 0 if piece < 2 else 1
        load_piece(K, xq, K.XS3, piece, tmpq, 'xq')
        cs = slice(piece * 256, (piece + 1) * 256)
        for c in range(NCH):
            S.op('dve', lambda e, c=c, cond=cond: e.tensor_scalar(ut, xq[:, c, :], K.modc(1, 4, c, cond), K.modc(1, 3, c, cond),
                                                                  ALU.mult, ALU.add), r=['xq', 'mod'], w=['ut'])
            S.op('act', lambda e, c=c, cs=cs: e.activation(uT[:, c, cs], ut, AF.Copy), r=['ut'], w=['uT'])
            S.op('dve', lambda e, c=c, cs=cs: e.tensor_tensor(loT[:, c, cs], ut, uT[:, c, cs], ALU.subtract), r=['ut', 'uT'], w=['loT'])
    for tb in range(NTB):
        ts_ = slice(tb * 128, (tb + 1) * 128)
        K.mm_group(ps[0][:, 0:8], [uT[:, k, ts_] for k in range(NCH)] + [loT[:, k, ts_] for k in range(NCH)] + [uT[:, k, ts_] for k in range(NCH)],
                   [rhi[:, k, :] for k in range(NCH)] * 2 + [rlo[:, k, :] for k in range(NCH)], r=['uT', 'loT', 'rhi', 'rlo'], w=['ps0'])
        S.op('act', lambda e: e.activation(lg, ps[0][:, 0:8], AF.Copy), r=['ps0'], w=['lg'])
        S.op('dve', lambda e: e.max(m8, lg), r=['lg'], w=['m8'])
        S.op('dve', lambda e: e.tensor_tensor(sm[:, 0:1], m8[:, 1:2], m8[:, 0:1], ALU.subtract), r=['m8'], w=['sm'])
        S.op('act', lambda e: e.activation(sm[:, 1:2], sm[:, 0:1], AF.Exp), r=['sm'], w=['sm'])
        S.op('dve', lambda e: e.tensor_scalar(sm[:, 2:3], sm[:, 1:2], 1.0, None, ALU.add), r=['sm'], w=['sm'])
        S.op('dve', lambda e: e.reciprocal(sm[:, 3:4], sm[:, 2:3]), r=['sm'], w=['sm'])
        S.op('dve', lambda e: e.tensor_tensor(sm[:, 4:5], sm[:, 1:2], sm[:, 3:4], ALU.mult), r=['sm'], w=['sm'])
        mk, cb_ = maskf[:, tb, :], combf[:, tb, :]
        S.op('dve', lambda e, mk=mk: e.tensor_scalar(mk, lg, m8[:, 0:1], None, ALU.is_equal), r=['lg', 'm8'], w=['maskf'])
        S.op('dve', lambda e: e.tensor_scalar(c2, lg, m8[:, 1:2], None, ALU.is_equal), r=['lg', 'm8'], w=['c2'])
        S.op('dve', lambda e, mk=mk, cb_=cb_: e.tensor_scalar(cb_, mk, sm[:, 3:4], None, ALU.mult), r=['maskf', 'sm'], w=['combf'])
        S.op('dve', lambda e, cb_=cb_: e.scalar_tensor_tensor(cb_, c2, sm[:, 4:5], cb_, ALU.mult, ALU.add), r=['c2', 'sm', 'combf'], w=['combf'])
        S.op('dve', lambda e, mk=mk: e.tensor_tensor(mk, mk, c2, ALU.add), r=['maskf', 'c2'], w=['maskf'])
        S.op('dve', lambda e, mk=mk, tb=tb: e.tensor_copy(maskb[:, tb, :], mk), r=['maskf'], w=['maskb'])
        for hf in range(2):
            def fn(e, hf=hf, ts_=ts_):
                ins = None
                for kk in range(8):
                    ins = e.transpose(psb[6 + hf][:, kk * 128:(kk + 1) * 128], uT[:, hf * 8 + kk, ts_], K.ident)
                return ins
            S.op('pe', fn, r=['uT', 'ident'], w=['ps%d' % (6 + hf)])
            S.op('act', lambda e, hf=hf, tb=tb: e.activation(u_tok[:, tb, hf * 1024:(hf + 1) * 1024], psb[6 + hf][:, 0:1024], AF.Copy),
                 r=['ps%d' % (6 + hf)], w=['u_tok'])
    for tb in range(NTB):
        K.mm_group(ps[1][:, tb * 8:(tb + 1) * 8], [K.ones] * tb + [tri], [maskb[:, j, :] for j in range(tb)] + [maskb[:, tb, :]],
                   r=['maskb', 'tri', 'ones'], w=['ps1'])
    S.op('dve', lambda e: e.tensor_copy(posf[0], ps[1][:, 0:NTB * 8].rearrange("p (a b) -> p a b", b=8)), r=['ps1'], w=['posf'])
    S.op('dve', lambda e: e.tensor_scalar(posf[1], posf[0], -512.0, None, ALU.add), r=['posf'], w=['posf'])
    K.mm_group(ps[2][:, 0:8], [K.ones] * NTB, [maskb[:, j, :] for j in range(NTB)], r=['maskb', 'ones'], w=['ps2'])
    S.op('dve', lambda e: e.tensor_copy(cntf, ps[2][:, 0:8]), r=['ps2'], w=['cntf'])
    S.op('dve', lambda e: e.tensor_copy(cnt_i, cntf), r=['cntf'], w=['cnt_i'])
    S.barrier()
    A.off = base
    SMAX = 512
    G1 = A.alloc((NTB, SMAX), BF16)
    Gc = A.alloc((NTB, SMAX), BF16)
    GcT = A.alloc((SMAX // 128, NMOE), BF16)
    ugT_off = A.off
    ugT = A.alloc((NCH, SMAX), BF16)
    accs = A.alloc((NCH, SMAX))
    accb = A.alloc((NCH, 128), BF16)
    Ys = A.at(ugT_off, (SMAX // 128, D), BF16)
    hbuf = [A.alloc((4, SMAX), BF16) for _ in range(2)]
    sbuf = [A.alloc(SMAX) for _ in range(2)]
    stg = [A.alloc(NMOE) for _ in range(2)]
    ws13 = K.WS('we13', 5, (NCH, 256))
    ws2 = K.WS('we2', 3, (2, D))
    halves = [(0, 384), (384, 384)]
    S.op('dve', lambda e: e.memset(stg[0], 0.0), w=['stg0'])
    for d in range(NCH):
        S.op('pool', lambda e, d=d: [e.dma_start(out=K.MO[:, d, :], in_=stg[0])], r=['stg0'], w=['MO%d' % d], chan='st_acc0')

    def expert_pass(ex, pidx, ns):
        nst = ns // 128
        for tb in range(NTB):
            S.op('dve', lambda e, tb=tb: e.tensor_scalar(G1[:, tb, 0:ns], iota[:, 0:ns], posf[pidx][:, tb, ex:ex + 1],
                                                         maskf[:, tb, ex:ex + 1], ALU.is_equal, ALU.mult), r=['iota'], w=['G1'])
            S.op('dve', lambda e, tb=tb: e.tensor_scalar(Gc[:, tb, 0:ns], iota[:, 0:ns], posf[pidx][:, tb, ex:ex + 1],
                                                         combf[:, tb, ex:ex + 1], ALU.is_equal, ALU.mult), r=['iota'], w=['Gc'])
        for k in range(NCH):
            b = k % 2
            K.mm_group(ps[b][:, 0:ns], [u_tok[:, tb, k * 128:(k + 1) * 128] for tb in range(NTB)],
                       [G1[:, tb, 0:ns] for tb in range(NTB)], r=['G1'], w=['ps%d' % b])
            S.op('act', lambda e, k=k, b=b: e.activation(ugT[:, k, 0:ns], ps[b][:, 0:ns], AF.Copy), r=['ps%d' % b], w=['ugT'])
        ffn_pass(K, ugT, 'ugT', [(0, ns)], I['exp_w1'][ex], I['exp_w3'][ex], I['exp_w2'][ex], DFFE, accs, 'accs',
                 ws13, ws2, hbuf, sbuf, True)
        for st_ in range(nst):
            S.op('act', lambda e, st_=st_: e.activation(accb, accs[:, :, st_ * 128:(st_ + 1) * 128], AF.Copy), r=['accs_%d' % d_ for d_ in range(NCH)], w=['accb'])
            for hf in range(2):
                def fn(e, hf=hf):
                    ins = None
                    for kk in range(8):
                        ins = e.transpose(psb[6][:, kk * 128:(kk + 1) * 128], accb[:, hf * 8 + kk, :], K.ident)
                    return ins
                S.op('pe', fn, r=['accb', 'ident'], w=['ps6'])
                S.op('act', lambda e, st_=st_, hf=hf: e.activation(Ys[:, st_, hf * 1024:(hf + 1) * 1024], psb[6][:, 0:1024], AF.Copy),
                     r=['ps6'], w=['ugT'])

            def fn2(e, st_=st_):
                ins = None
                for tb in range(NTB):
                    ins = e.transpose(psb[7][:, tb * 128:(tb + 1) * 128], Gc[:, tb, st_ * 128:(st_ + 1) * 128], K.ident)
                return ins
            S.op('pe', fn2, r=['Gc', 'ident'], w=['ps7'])
            S.op('act', lambda e, st_=st_: e.activation(GcT[:, st_, :], psb[7][:, 0:NMOE], AF.Copy), r=['ps7'], w=['GcT'])
        for d in range(NCH):
            sg, sk = stg[d % 2], 'stg%d' % (d % 2)
            for hi_, (c0, n) in enumerate(halves):
                b = 4 + hi_
                K.mm_group(ps[b][:, 0:n], [Ys[:, st_, d * 128:(d + 1) * 128] for st_ in range(nst)],
                           [GcT[:, st_, c0:c0 + n] for st_ in range(nst)], r=['ugT', 'GcT'], w=['ps%d' % b])
                if hi_ == 0:
                    S.op('act', lambda e, b=b, sg=sg, c0=c0, n=n: e.activation(sg[:, c0:c0 + n], ps[b][:, 0:n], AF.Copy),
                         r=['ps%d' % b], w=[sk])
                else:
                    S.op('dve', lambda e, b=b, sg=sg, c0=c0, n=n: e.tensor_copy(sg[:, c0:c0 + n], ps[b][:, 0:n]),
                         r=['ps%d' % b], w=[sk])
            S.op('pool', lambda e, d=d, sg=sg: [e.dma_start(out=K.MO[:, d, :], in_=sg, accum_op=ALU.add)],
                 r=[sk], w=['MO%d' % d], chan='st_acc%d' % (d % 2))

    for ex in range(NEXP):
        S.branch_begin((cnt_i[0:1, ex:ex + 1], list(MOE_THR)))
        expert_pass(ex, 0, 256)
        S.branch_next()
        expert_pass(ex, 0, 512)
        S.branch_next()
        expert_pass(ex, 0, 512)
        expert_pass(ex, 1, 256)
        S.branch_end()
```

```python
import math
import numpy as np
import concourse.bass as bass
import concourse.mybir as mybir
from concourse.bass_utils import run_bass_kernel_spmd

F32 = mybir.dt.float32
BF16 = mybir.dt.bfloat16
AF = mybir.ActivationFunctionType
ALU = mybir.AluOpType
AX = mybir.AxisListType


class Sched:
    ENG = ('pe', 'act', 'dve', 'pool', 'sp')

    def __init__(self):
        self.ops = []
        self.lastw = {}
        self.readers = {}
        self.cnt = {e: 0 for e in self.ENG}
        self.chan_cnt = {}
        self.chan_eng = {}
        self.bar_pending = {}
        self.branches = []
        self.cur = None

    def op(self, eng, fn, r=(), w=(), chan=None, ndma=1):
        deps = set()
        for k in r:
            if k in self.lastw:
                deps.add(self.lastw[k])
        for k in w:
            if k in self.lastw:
                deps.add(self.lastw[k])
            deps.update(self.readers.get(k, ()))
        deps |= self.bar_pending.pop(eng, set())
        if chan is None:
            self.cnt[eng] += 1
            sig = ('e', eng, self.cnt[eng])
        else:
            self.chan_cnt[chan] = self.chan_cnt.get(chan, 0) + 16 * ndma
            self.chan_eng[chan] = eng
            sig = ('c', chan, self.chan_cnt[chan])
        self.ops.append(dict(eng=eng, fn=fn, deps=deps, sig=sig, chan=chan, br=self.cur))
        for k in r:
            self.readers.setdefault(k, []).append(sig)
        for k in w:
            self.lastw[k] = sig
            self.readers[k] = []
        return sig

    def _all_signals(self):
        s = {('e', e, v) for e, v in self.cnt.items() if v > 0}
        s |= {('c', c, v) for c, v in self.chan_cnt.items() if v > 0}
        return s

    def barrier(self):
        sig = self._all_signals()
        for e in self.ENG:
            self.bar_pending[e] = set(sig)
        self.lastw = {}
        self.readers = {}

    def branch_begin(self, cond_fn):
        assert self.cur is None
        self.barrier()
        b = dict(cond_fn=cond_fn, base_cnt=dict(self.cnt), base_chan=dict(self.chan_cnt),
                 bar=self._all_signals(), ends=[])
        self.branches.append(b)
        self.cur = (len(self.branches) - 1, 0)

    def _end_path(self):
        b = self.branches[self.cur[0]]
        b['ends'].append((dict(self.cnt), dict(self.chan_cnt)))

    def branch_next(self):
        self._end_path()
        bi, pi = self.cur
        b = self.branches[bi]
        self.cnt = dict(b['base_cnt'])
        self.chan_cnt = dict(b['base_chan'])
        self.lastw, self.readers = {}, {}
        self.bar_pending = {e: set(b['bar']) for e in self.ENG}
        self.cur = (bi, pi + 1)

    def branch_end(self):
        self._end_path()
        b = self.branches[self.cur[0]]
        cnt = {}
        for e in self.ENG:
            cnt[e] = max(c[e] for c, _ in b['ends'])
        chans = set()
        for _, cc in b['ends']:
            chans |= set(cc)
        chan = {c: max(cc.get(c, 0) for _, cc in b['ends']) for c in chans}
        b['final_cnt'], b['final_chan'] = cnt, chan
        self.cnt, self.chan_cnt = dict(cnt), dict(chan)
        self.cur = None
        self.barrier()

    def finish(self):
        self.barrier()
        self.op('sp', None)

    def emit(self, eng_name, eng, sems):
        waited = {}

        def emit_op(o):
            for (kind, key, val) in sorted(o['deps']):
                if eng_name == 'pe' and kind == 'e' and key == 'pe':
                    continue
                if waited.get((kind, key), 0) >= val:
                    continue
                eng.wait_ge(sems[(kind, key)], val)
                waited[(kind, key)] = val
            if o['fn'] is None:
                return
            res = o['fn'](eng)
            if o['chan'] is not None:
                for ins in res:
                    ins.then_inc(sems[('c', o['chan'])], 16)
            else:
                res.then_inc(sems[('e', eng_name)], 1)

        def pad(b, pi):
            end_cnt, end_chan = b['ends'][pi]
            need = b['final_cnt'][eng_name] - end_cnt[eng_name]
            if need > 0:
                eng.wait_ge(sems[('e', eng_name)], end_cnt[eng_name])
                eng.sem_inc(sems[('e', eng_name)], need)
            for c, fin in b['final_chan'].items():
                if self.chan_eng.get(c) != eng_name:
                    continue
                have = end_chan.get(c, 0)
                if fin - have > 0:
                    eng.wait_ge(sems[('c', c)], have)
                    eng.sem_inc(sems[('c', c)], fin - have)

        mine = [o for o in self.ops if o['eng'] == eng_name]
        done_br = set()
        for o in mine:
            if o['br'] is None:
                emit_op(o)
                continue
            bi = o['br'][0]
            if bi in done_br:
                continue
            done_br.add(bi)
            b = self.branches[bi]
            npaths = len(b['ends'])
            w0 = dict(waited)
            for (kind, key, val) in sorted(b['bar']):
                if waited.get((kind, key), 0) < val and not (eng_name == 'pe' and kind == 'e' and key == 'pe'):
                    eng.wait_ge(sems[(kind, key)], val)
                    waited[(kind, key)] = val
            w1 = dict(waited)
            cond_ap, thrs = b['cond_fn']
            if not isinstance(thrs, (list, tuple)):
                thrs = [thrs]
            assert npaths == len(thrs) + 1

            def emit_path(pi):
                waited.clear()
                waited.update(w1)
                for oo in mine:
                    if oo['br'] == (bi, pi):
                        emit_op(oo)
                pad(b, pi)

            def nest(pi):
                if pi == npaths - 1:
                    emit_path(pi)
                    return
                with eng.If_lt(cr, thrs[pi]):
                    emit_path(pi)
                with eng.Else():
                    nest(pi + 1)
            with eng.register("br%d_%s" % (bi, eng_name)) as cr:
                eng.reg_load(cr, cond_ap)
                nest(0)
            waited.clear()
            waited.update(w1)


class Arena:
    def __init__(self, ap_f32, nwords):
        self.ap = ap_f32
        self.n = nwords
        self.off = 0
        self.mark = 0

    def alloc(self, free_shape, dtype=F32):
        if isinstance(free_shape, int):
            free_shape = (free_shape,)
        n = int(np.prod(free_shape))
        words = n if dtype == F32 else (n + 1) // 2
        assert self.off + words <= self.n, ("arena overflow", self.off, words, self.n)
        a = self.ap[:, self.off:self.off + words]
        self.off += words
        if dtype != F32:
            a = a.bitcast(dtype)
        if len(free_shape) == 2:
            a = a.rearrange("p (a b) -> p a b", b=free_shape[1])
        elif len(free_shape) == 3:
            a = a.rearrange("p (a b c) -> p a b c", b=free_shape[1], c=free_shape[2])
        return a

    def at(self, off, free_shape, dtype=F32):
        save = self.off
        self.off = off
        a = self.alloc(free_shape, dtype)
        self.off = save
        return a

    def set_mark(self):
        self.mark = self.off

    def reset(self):
        self.off = self.mark


D = 2048
NCH = 16
LP, LS = 256, 1024
NCOL = 1536
NMOE = 768
DFF = 5632
DFFE = 7168
NEXP = 8
ALPHA = 4.0 ** 0.25
LN_EPS = 1e-5
QK_EPS = 1e-6
ARENA_WORDS = 47104
MAGIC = 12582912.0

PK = {}
_o = 0
for _n, _w in [('ada_b0', 96), ('ada_b1', 96), ('ln', 128), ('conv_w', 32), ('conv_b', 8), ('lam', 16),
               ('b_r', 16), ('b_i', 16), ('h0', 16), ('short_w', 144), ('short_b', 48), ('fbias', 32),
               ('qmask', 4)]:
    PK[_n] = (_o, _w)
    _o += _w
NPK = _o


class Ctx:
    pass


def build_program(debug=False):
    nc = bass.Bass("TRN2", target_bir_lowering=False)
    K = Ctx()
    K.nc = nc
    S = K.S = Sched()

    def din(name, shape):
        return nc.dram_tensor(name, list(shape), F32, kind="ExternalInput").ap()

    def dout(name, shape):
        return nc.dram_tensor(name, list(shape), F32, kind="ExternalOutput").ap()

    def dscr(name, shape):
        return nc.dram_tensor(name, list(shape), F32, kind="ExternalOutput" if debug else "Internal").ap()

    I = {}
    for name, shape in [
        ('xT', (128, NCH, NCOL)), ('condT', (128, NCH, 2)), ('pk', (128, NPK)), ('qg', (128, 128)), ('kg', (128, 128)),
        ('rope', (128, 8, 2, 64)), ('ckT', (128, 2, 256)), ('cv', (128, 2, 256)),
        ('l0_ada_w', (24, 128, NCH, 512)), ('l0_w_in', (7, 128, NCH, 512)), ('lru_wr', (128, 2, 8, 128)), ('lru_wi', (128, 2, 8, 128)),
        ('l0_w_out', (4, 128, NCH, 512)), ('l0_ffn_w1', (22, 128, NCH, 256)), ('l0_ffn_w3', (22, 128, NCH, 256)), ('l0_ffn_w2', (DFF, D)),
        ('l1_ada_w', (24, 128, NCH, 512)), ('l1_w_in', (48, 128, NCH, 128)), ('l1_w_out', (4, 128, NCH, 512)),
        ('router', (128, NCH, 8)), ('exp_w1', (NEXP, 28, 128, NCH, 256)), ('exp_w3', (NEXP, 28, 128, NCH, 256)), ('exp_w2', (NEXP, DFFE, D)),
        ('featT256', (33, 256)), ('featT1024', (33, 1024)), ('fw1', (33, 64)), ('fw2', (64, 64)), ('fw3', (64, 8192)),
        ('pk64', (64, 4)), ('decay', (1, 8192)), ('nt01_256', (128, 2)), ('nt01_1024', (128, 8)),
        ('dft256', (5, 256, 256)), ('dft1024', (5, 1024, 1024)),
        ('ident', (128, 128)), ('sel', (8, 8 * 128)), ('tri', (128, 128)), ('iota', (128, 512)),
    ]:
        I[name] = din(name, shape)
    O = {}
    O['yT'] = dout('yT', (128, NCH, NMOE))
    O['kout'] = dout('kout', (512, 256))
    O['vout'] = dout('vout', (512, 256))
    O['hout'] = dout('hout', (128, 32))
    XS1 = dscr('XS1', (128, NCH, NCOL))
    XS2 = dscr('XS2', (128, NCH, NCOL))
    XS3 = dscr('XS3', (128, NCH, NCOL))
    MO = dscr('MO', (128, NCH, NMOE))
    KF = {256: dscr('KF256', (3, 2, 128, 2 * D)), 1024: dscr('KF1024', (3, 8, 128, 2 * D))}
    ZZ = dscr('ZZ', (128, NCH, NCOL))

    import contextlib
    st = contextlib.ExitStack()
    arena_t = st.enter_context(nc.sbuf_tensor("arena", [128, ARENA_WORDS], F32))
    ps = [st.enter_context(nc.psum_tensor("ps%d" % i, [128, 512], F32))[:] for i in range(8)]
    psb = [p.bitcast(BF16) for p in ps]
    A = Arena(arena_t[:], ARENA_WORDS)

    pk = A.alloc(NPK)
    mod = [A.alloc((96, 2)), A.alloc((96, 2))]
    ident = A.alloc(128, BF16)
    ones = A.alloc(128, BF16)
    c8 = A.alloc(16)
    hst = A.alloc(32)
    A.set_mark()

    def pkc(name, i=0, n=1):
        o, w = PK[name]
        return pk[:, o + i:o + i + n]

    def modc(l, part, c, cond):
        return mod[l][:, part * 16 + c, cond:cond + 1]

    uid = [0]

    def U(prefix):
        uid[0] += 1
        return "%s#%d" % (prefix, uid[0])

    class WS:
        def __init__(self, name, nslots, free_shape):
            self.name, self.n, self.i = name, nslots, 0
            self.slots = [A.alloc(free_shape, BF16) for _ in range(nslots)]

        def load(self, src_ap, sub=None):
            s = self.i % self.n
            self.i += 1
            dst = self.slots[s] if sub is None else sub(self.slots[s])
            key = "%s_%d" % (self.name, s)
            S.op('pool', lambda e: [e.dma_start(out=dst, in_=src_ap, max_dma_last_dim=8192)], w=[key], chan=key)
            return self.slots[s], key

    def wunit(w_ap, c0, ncols):
        assert c0 % ncols == 0 and w_ap.shape[-1] == ncols, (w_ap.shape, c0, ncols)
        return w_ap[c0 // ncols]

    def mm_group(out_ps, lhs_list, rhs_list, r, w):
        n = len(lhs_list)

        def fn(e):
            ins = None
            for i in range(n):
                ins = e.matmul(out_ps, lhs_list[i], rhs_list[i], start=(i == 0), stop=(i == n - 1))
            return ins
        S.op('pe', fn, r=r, w=w)

    S.op('sp', lambda e: [e.dma_start(out=pk, in_=I['pk'])], w=['pk'], chan='ld_pk')
    S.op('pool', lambda e: [e.dma_start(out=ident, in_=I['ident'])], w=['ident'], chan='ld_ident')
    S.op('dve', lambda e: e.memset(ones, 1.0), w=['ones'])
    S.op('act', lambda e: e.activation(c8, pkc('lam', 0, 16), AF.Exp, scale=-1.0), r=['pk'], w=['c8'])
    S.op('act', lambda e: e.activation(c8, c8, AF.Ln, bias=1.0), r=['c8'], w=['c8'])
    S.op('dve', lambda e: e.tensor_scalar(c8, c8, -8.0, None, ALU.mult), r=['c8'], w=['c8'])

    def phase_ada():
        condT = A.alloc((NCH, 2))
        sT = A.alloc((NCH, 2), BF16)
        sg = A.alloc((NCH, 2))
        S.op('sp', lambda e: [e.dma_start(out=condT, in_=I['condT'])], w=['condT'], chan='ld_cond')
        S.op('act', lambda e: e.activation(sg, condT, AF.Silu), r=['condT'], w=['sg'])
        S.op('dve', lambda e: e.tensor_copy(sT, sg), r=['sg'], w=['sT'])
        ws = WS('wa', 3, (NCH, 512))
        for l in range(2):
            wname = 'l%d_ada_w' % l
            for u in range(24):
                slot, key = ws.load(wunit(I[wname], u * 512, 512))
                bank = ps[u % 2]
                bk = 'ps%d' % (u % 2)
                for m in range(4):
                    mm_group(bank[:, m * 2:m * 2 + 2], [slot[:, k, m * 128:(m + 1) * 128] for k in range(NCH)],
                             [sT[:, k, :] for k in range(NCH)], r=[key, 'sT'], w=[bk])
                c0 = u * 4
                bo = PK['ada_b%d' % l][0]
                S.op('dve', lambda e, bank=bank, l=l, c0=c0, bo=bo: e.tensor_tensor(
                    mod[l][:, c0:c0 + 4, :], bank[:, 0:8].rearrange("p (a b) -> p a b", b=2),
                    pk[:, bo + c0:bo + c0 + 4].unsqueeze(2).to_broadcast([128, 4, 2]), ALU.add),
                    r=[bk, 'pk'], w=['mod%d' % l])
            for part in (1, 4):
                S.op('dve', lambda e, l=l, part=part: e.tensor_scalar(
                    mod[l][:, part * 16:(part + 1) * 16, :], mod[l][:, part * 16:(part + 1) * 16, :], 1.0, None, ALU.add),
                    r=['mod%d' % l], w=['mod%d' % l])

    phase_ada()
    S.barrier()
    A.reset()

    K.__dict__.update(dict(I=I, O=O, A=A, ps=ps, psb=psb, pk=pk, mod=mod, ident=ident, ones=ones, c8=c8, hst=hst,
                           pkc=pkc, modc=modc, WS=WS, wunit=wunit, mm_group=mm_group, XS1=XS1, XS2=XS2, XS3=XS3,
                           MO=MO, KF=KF, ZZ=ZZ, st=st, U=U))
    return K


def load_x_tile(K, dst, src, c0, n, key, chan):
    K.S.op('sp', lambda e: [e.dma_start(out=dst, in_=src[:, :, c0:c0 + n])], w=[key], chan=chan)


def modulate_to_bf16(K, l, pshift, pscale, cond_of_col, xt, xkey, uT, ukey, c0s):
    S = K.S
    for c in range(NCH):
        for (a, n, cond) in c0s:
            S.op('dve', lambda e, c=c, a=a, n=n, cond=cond: e.tensor_scalar(
                uT[:, c, a:a + n], xt[:, c, a:a + n], K.modc(l, pscale, c, cond), K.modc(l, pshift, c, cond),
                ALU.mult, ALU.add), r=[xkey, 'mod'], w=[ukey])


def post_norm(K, l, pgate, ln_idx, xt, xkey, delta_fn, conds, ncols, tmpA):
    S = K.S
    ps = K.ps
    tmp = tmpA.alloc(ncols)
    yb = [tmpA.alloc(ncols, BF16) for _ in range(2)]
    ysq = [tmpA.alloc(ncols, BF16) for _ in range(2)]
    mean = tmpA.alloc(ncols)
    rstd = tmpA.alloc(ncols)
    k_tmp, k_mean, k_rstd = 'pn_tmp', 'pn_mean', 'pn_rstd'
    k_yb = ['pn_yb0', 'pn_yb1']
    k_ysq = ['pn_ysq0', 'pn_ysq1']
    for m in range(NCH):
        dap, dkey = delta_fn(m)
        for (a, n, cond) in conds:
            S.op('act', lambda e, a=a, n=n, cond=cond, dap=dap, m=m: e.activation(
                tmp[:, a:a + n], dap[:, a:a + n], AF.Identity, scale=K.modc(l, pgate, m, cond)),
                r=[dkey, 'mod'], w=[k_tmp])
        S.op('dve', lambda e, m=m: e.scalar_tensor_tensor(xt[:, m, 0:ncols], xt[:, m, 0:ncols], ALPHA, tmp[:, 0:ncols],
                                                          ALU.mult, ALU.add), r=[k_tmp, xkey], w=[xkey])
        b = m % 2
        S.op('act', lambda e, m=m, b=b: e.activation(ysq[b], xt[:, m, 0:ncols], AF.Square), r=[xkey], w=[k_ysq[b]])
        S.op('dve', lambda e, m=m, b=b: e.tensor_copy(yb[b], xt[:, m, 0:ncols]), r=[xkey], w=[k_yb[b]])
        S.op('pe', lambda e, m=m, b=b: e.matmul(ps[6][:, 0:ncols], K.ones, yb[b], start=(m == 0), stop=(m == NCH - 1)),
             r=[k_yb[b], 'ones'], w=['ps6'])
        S.op('pe', lambda e, m=m, b=b: e.matmul(ps[7][:, 0:ncols], K.ones, ysq[b], start=(m == 0), stop=(m == NCH - 1)),
             r=[k_ysq[b], 'ones'], w=['ps7'])
    S.op('dve', lambda e: e.tensor_scalar(mean, ps[6][:, 0:ncols], 1.0 / D, None, ALU.mult), r=['ps6'], w=[k_mean])
    S.op('dve', lambda e: e.tensor_tensor(tmp, mean, mean, ALU.mult), r=[k_mean], w=[k_tmp])
    S.op('dve', lambda e: e.scalar_tensor_tensor(rstd, ps[7][:, 0:ncols], 1.0 / D, tmp, ALU.mult, ALU.subtract),
         r=['ps7', k_tmp], w=[k_rstd])
    S.op('act', lambda e: e.activation(rstd, rstd, AF.Sqrt, bias=LN_EPS), r=[k_rstd], w=[k_rstd])
    S.op('dve', lambda e: e.reciprocal(rstd, rstd), r=[k_rstd], w=[k_rstd])
    go = PK['ln'][0] + ln_idx * 32
    for m in range(NCH):
        S.op('dve', lambda e, m=m: e.tensor_tensor(xt[:, m, 0:ncols], xt[:, m, 0:ncols], mean, ALU.subtract),
             r=[xkey, k_mean], w=[xkey])
        S.op('dve', lambda e, m=m: e.tensor_tensor(xt[:, m, 0:ncols], xt[:, m, 0:ncols], rstd, ALU.mult),
             r=[xkey, k_rstd], w=[xkey])
        S.op('dve', lambda e, m=m: e.tensor_scalar(xt[:, m, 0:ncols], xt[:, m, 0:ncols], K.pk[:, go + m:go + m + 1],
                                                   K.pk[:, go + 16 + m:go + 17 + m], ALU.mult, ALU.add),
             r=[xkey, 'pk'], w=[xkey])


def wout_postnorm(K, l, w_ap, catT, catkey, col0, ncols, conds, src, dst, ws, gate_part, ln_idx, tagp, xt=None):
    S, A, ps = K.S, K.A, K.ps
    if xt is None:
        xt = A.alloc((NCH, ncols))
    xkey = 'xt'
    load_x_tile(K, xt, src, col0, ncols, xkey, 'ld_x_' + tagp)
    state = {}

    def delta_fn(m):
        u, mm = divmod(m, 4)
        if mm == 0:
            state['slot'], state['key'] = ws.load(K.wunit(w_ap, u * 512, 512))
        slot, key = state['slot'], state['key']
        bank = ps[m % 2]
        K.mm_group(bank[:, 0:ncols], [slot[:, k, mm * 128:(mm + 1) * 128] for k in range(NCH)],
                   [catT[:, k, col0:col0 + ncols] for k in range(NCH)], r=[key, catkey], w=['ps%d' % (m % 2)])
        return bank, 'ps%d' % (m % 2)
    post_norm(K, l, gate_part, ln_idx, xt, xkey, delta_fn, conds, ncols, A)
    S.op('sp', lambda e: [e.dma_start(out=dst[:, :, col0:col0 + ncols], in_=xt)], r=[xkey], chan='st_x_' + tagp)


def phase_mixer0(K, grp):
    S, A, I, O, ps, psb = K.S, K.A, K.I, K.O, K.ps, K.psb
    if grp == 'P':
        col0, ncols, nseg, L, cond, koff = 0, 512, 2, 256, 0, 0
    else:
        col0, ncols, nseg, L, cond, koff = 512, 1024, 1, 1024, 1, 256
    ntb = ncols // 128
    nkb_seg = (L + koff) // 128
    conds = [(0, ncols, cond)]
    uT_off = A.off
    uT = A.alloc((NCH, ncols), BF16)
    catT = A.alloc((NCH, ncols), BF16)
    qg = A.alloc(128)
    kg = A.alloc(128)
    wr = A.alloc((2, 8, 128), BF16)
    wi = A.alloc((2, 8, 128), BF16)
    S.op('sp', lambda e: [e.dma_start(out=qg, in_=I['qg'])], w=['qg'], chan='ld_qg')
    S.op('sp', lambda e: [e.dma_start(out=kg, in_=I['kg'])], w=['kg'], chan='ld_kg')
    S.op('pool', lambda e: [e.dma_start(out=wr, in_=I['lru_wr'])], w=['wr'], chan='ld_wr')
    S.op('pool', lambda e: [e.dma_start(out=wi, in_=I['lru_wi'])], w=['wi'], chan='ld_wi')
    if grp == 'S':
        rope = A.alloc((8, 2, 64))
        S.op('sp', lambda e: [e.dma_start(out=rope, in_=I['rope'])], w=['rope'], chan='ld_rope')
    ws = K.WS('wm', 2, (NCH, 512))
    base = A.off
    xtmp = A.alloc((NCH, 512))
    for t in range(ncols // 512):
        load_x_tile(K, xtmp, I['xT'], col0 + t * 512, 512, 'xtmp', 'ld_xtmp')
        for c in range(NCH):
            S.op('dve', lambda e, c=c, t=t: e.tensor_scalar(
                uT[:, c, t * 512:(t + 1) * 512], xtmp[:, c, :], K.modc(0, 1, c, cond), K.modc(0, 0, c, cond),
                ALU.mult, ALU.add), r=['xtmp', 'mod'], w=['uT'])
    S.barrier()
    A.off = base
    qT = A.alloc((8, ncols), BF16)
    kT = A.alloc((2, koff + ncols), BF16)
    V = A.alloc((koff // 128 + ntb, 256), BF16)
    if grp == 'S':
        S.op('pool', lambda e: [e.dma_start(out=kT[:, :, 0:256], in_=I['ckT'])], w=['kT'], chan='ld_ck')
        S.op('pool', lambda e: [e.dma_start(out=V[:, 0:2, :], in_=I['cv'])], w=['V'], chan='ld_cv')
    qf = A.alloc(512)
    sq = A.alloc(512)
    qr = A.alloc(512)
    ss = A.alloc(4)
    qb = A.alloc(512, BF16)
    kn = [A.alloc(256), A.alloc(256)]
    vf = [A.alloc(256), A.alloc(256)]

    def norm_heads(nh, gain, gkey, dst_f32):
        S.op('dve', lambda e: e.tensor_tensor(sq[:, 0:nh * 128], qf[:, 0:nh * 128], qf[:, 0:nh * 128], ALU.mult),
             r=['qf'], w=['sq'])
        S.op('dve', lambda e: e.tensor_reduce(ss[:, 0:nh], sq[:, 0:nh * 128].rearrange("p (a b) -> p a b", b=128),
                                              AX.X, ALU.add), r=['sq'], w=['ss'])
        S.op('act', lambda e: e.activation(ss[:, 0:nh], ss[:, 0:nh], AF.Sqrt, scale=1.0 / 128, bias=QK_EPS),
             r=['ss'], w=['ss'])
        S.op('dve', lambda e: e.reciprocal(ss[:, 0:nh], ss[:, 0:nh]), r=['ss'], w=['ss'])
        for h in range(nh):
            S.op('dve', lambda e, h=h: e.scalar_tensor_tensor(
                dst_f32[:, h * 128:(h + 1) * 128], qf[:, h * 128:(h + 1) * 128], ss[:, h:h + 1], gain,
                ALU.mult, ALU.mult), r=['qf', 'ss', gkey], w=['qn'])

    def rope_to_bf16(nh, src_f32, tb):
        if grp == 'P':
            S.op('dve', lambda e: e.tensor_copy(qb[:, 0:nh * 128], src_f32[:, 0:nh * 128]), r=['qn'], w=['qb'])
            return
        cos = rope[:, tb, 0, :]
        sin = rope[:, tb, 1, :]
        sv = src_f32[:, 0:nh * 128].rearrange("p (h i two) -> p h i two", i=64, two=2)
        qv = qb[:, 0:nh * 128].rearrange("p (h i two) -> p h i two", i=64, two=2)
        t0 = sq[:, 0:nh * 64].rearrange("p (h i) -> p h i", i=64)
        t1 = sq[:, 256:256 + nh * 64].rearrange("p (h i) -> p h i", i=64)
        cb = cos.unsqueeze(1).to_broadcast([128, nh, 64])
        sb = sin.unsqueeze(1).to_broadcast([128, nh, 64])
        x0 = sv[:, :, :, 0]
        x1 = sv[:, :, :, 1]
        S.op('dve', lambda e: e.tensor_tensor(t0, x0, cb, ALU.mult), r=['qn', 'rope'], w=['sq'])
        S.op('dve', lambda e: e.tensor_tensor(t1, x1, sb, ALU.mult), r=['qn', 'rope'], w=['sq'])
        S.op('dve', lambda e: e.tensor_tensor(qv[:, :, :, 0], t0, t1, ALU.subtract), r=['sq'], w=['qb'])
        S.op('dve', lambda e: e.tensor_tensor(t0, x0, sb, ALU.mult), r=['qn', 'rope', 'qb'], w=['sq'])
        S.op('dve', lambda e: e.tensor_tensor(t1, x1, cb, ALU.mult), r=['qn', 'rope'], w=['sq'])
        S.op('dve', lambda e: e.tensor_tensor(qv[:, :, :, 1], t0, t1, ALU.add), r=['sq'], w=['qb'])

    def transpose_heads(nh, dstT, h0, c0, dkey):
        def fn(e):
            ins = None
            for h in range(nh):
                ins = e.transpose(psb[6][:, h * 128:(h + 1) * 128], qb[:, h * 128:(h + 1) * 128], K.ident)
            return ins
        S.op('pe', fn, r=['qb', 'ident'], w=['ps6'])
        S.op('act', lambda e: e.activation(dstT[:, h0:h0 + nh, c0:c0 + 128],
                                           psb[6][:, 0:nh * 128].rearrange("p (h t) -> p h t", t=128), AF.Copy),
             r=['ps6'], w=[dkey])

    for u in range(3):
        slot, wkey = ws.load(K.wunit(I['l0_w_in'], u * 512, 512))
        for tb in range(ntb):
            bank, bk = ps[tb % 2], 'ps%d' % (tb % 2)
            K.mm_group(bank, [uT[:, k, tb * 128:(tb + 1) * 128] for k in range(NCH)], [slot[:, k, :] for k in range(NCH)],
                       r=[wkey, 'uT'], w=[bk])
            if u < 2:
                S.op('act', lambda e, bank=bank: e.activation(qf, bank, AF.Copy), r=[bk], w=['qf'])
                norm_heads(4, qg, 'qg', qr)
                rope_to_bf16(4, qr, tb)
                transpose_heads(4, qT, 4 * u, tb * 128, 'qT')
            else:
                kb = tb % 2
                S.op('act', lambda e, bank=bank: e.activation(qf[:, 0:256], bank[:, 0:256], AF.Copy), r=[bk], w=['qf'])
                S.op('act', lambda e, bank=bank, kb=kb: e.activation(vf[kb], bank[:, 256:512], AF.Copy), r=[bk],
                     w=['vf%d' % kb])
                norm_heads(2, kg, 'kg', kn[kb])
                rope_to_bf16(2, kn[kb], tb)
                transpose_heads(2, kT, 0, koff + tb * 128, 'kT')
                S.op('dve', lambda e, kb=kb, tb=tb: e.tensor_copy(V[:, koff // 128 + tb, :], vf[kb]),
                     r=['vf%d' % kb], w=['V'])
                if grp == 'P':
                    S.op('sp', lambda e, kb=kb, tb=tb: [e.dma_start(out=O['kout'][tb * 128:(tb + 1) * 128, :], in_=kn[kb])],
                         r=['qn'], chan='st_k%d' % kb)
                    S.op('sp', lambda e, kb=kb, tb=tb: [e.dma_start(out=O['vout'][tb * 128:(tb + 1) * 128, :], in_=vf[kb])],
                         r=['vf%d' % kb], chan='st_v%d' % kb)
    Eb = [A.alloc(512, BF16), A.alloc(512, BF16)]
    rden = A.alloc(512)
    it = 0
    sc = 128.0 ** -0.5
    for seg in range(nseg):
        kbase = seg * L if grp == 'P' else 0
        vbase = seg * (L // 128) if grp == 'P' else 0
        for h2 in range(2):
            for qb_i in range(L // 128):
                qc = seg * L + qb_i * 128
                rhs_q = qT[:, 4 * h2:4 * h2 + 4, qc:qc + 128]
                for kb in range(nkb_seg):
                    sb_, sk = ps[2 + it % 2], 'ps%d' % (2 + it % 2)
                    eb, ek = Eb[it % 2], 'E%d' % (it % 2)
                    it += 1
                    S.op('pe', lambda e, sb_=sb_, kb=kb, h2=h2, rhs_q=rhs_q, kbase=kbase: e.matmul(
                        sb_, kT[:, h2, kbase + kb * 128:kbase + (kb + 1) * 128], rhs_q, start=True, stop=True),
                        r=['kT', 'qT'], w=[sk])
                    S.op('act', lambda e, sb_=sb_, eb=eb: e.activation(eb, sb_, AF.Exp, scale=sc), r=[sk], w=[ek])
                    first, last = kb == 0, kb == nkb_seg - 1
                    S.op('pe', lambda e, eb=eb, kb=kb, h2=h2, first=first, last=last, vbase=vbase: e.matmul(
                        ps[4], V[:, vbase + kb, h2 * 128:(h2 + 1) * 128], eb, start=first, stop=last),
                        r=[ek, 'V'], w=['ps4'])
                    S.op('pe', lambda e, eb=eb, first=first, last=last: e.matmul(ps[5], K.ones, eb, start=first, stop=last),
                         r=[ek, 'ones'], w=['ps5'])
                S.op('dve', lambda e: e.reciprocal(rden, ps[5]), r=['ps5'], w=['rden'])
                S.op('dve', lambda e, h2=h2, qc=qc: e.tensor_tensor(
                    catT[:, 4 * h2:4 * h2 + 4, qc:qc + 128], ps[4].rearrange("p (h t) -> p h t", t=128),
                    rden.rearrange("p (h t) -> p h t", t=128), ALU.mult), r=['ps4', 'rden'], w=['catT'])
    S.barrier()
    A.off = base
    pb = A.alloc((nseg, L + 3))
    xc = A.alloc(ncols)
    xcb = A.alloc(ncols, BF16)
    gbf = A.alloc(ncols)
    t1 = A.alloc(ncols)
    t2 = A.alloc(ncols)
    aa = A.alloc(ncols)
    bb = A.alloc(ncols)
    hh = [A.alloc(ncols), A.alloc(ncols)]
    nb = ncols // 512
    S.op('dve', lambda e: e.memset(pb, 0.0), w=['pb'])
    cwo, cbo = PK['conv_w'][0], PK['conv_b'][0]
    seg3 = lambda ap: ap.rearrange("p (s l) -> p s l", l=L)
    for uu in range(2):
        sx, kx = ws.load(K.wunit(I['l0_w_in'], 1536 + uu * 512, 512))
        sg_, kg_ = ws.load(K.wunit(I['l0_w_in'], 2560 + uu * 512, 512))
        for jj in range(4):
            j = uu * 4 + jj
            for b in range(nb):
                K.mm_group(ps[b], [sx[:, k, jj * 128:(jj + 1) * 128] for k in range(NCH)],
                           [uT[:, k, b * 512:(b + 1) * 512] for k in range(NCH)], r=[kx, 'uT'], w=['ps%d' % b])
                K.mm_group(ps[2 + b], [sg_[:, k, jj * 128:(jj + 1) * 128] for k in range(NCH)],
                           [uT[:, k, b * 512:(b + 1) * 512] for k in range(NCH)], r=[kg_, 'uT'], w=['ps%d' % (2 + b)])
                if grp == 'P':
                    S.op('act', lambda e, b=b: e.activation(pb[:, :, 1:L + 1], seg3(ps[b]), AF.Copy), r=['ps%d' % b], w=['pb'])
                else:
                    S.op('act', lambda e, b=b: e.activation(pb[:, 0, 1 + b * 512:1 + (b + 1) * 512], ps[b], AF.Copy),
                         r=['ps%d' % b], w=['pb'])
                S.op('act', lambda e, b=b: e.activation(gbf[:, b * 512:(b + 1) * 512], ps[2 + b], AF.Copy),
                     r=['ps%d' % (2 + b)], w=['gbf'])
            xc3 = seg3(xc)
            S.op('dve', lambda e, j=j: e.tensor_scalar(xc3, pb[:, :, 0:L], K.pk[:, cwo + j * 4:cwo + j * 4 + 1],
                                                       K.pk[:, cbo + j:cbo + j + 1], ALU.mult, ALU.add),
                 r=['pb', 'pk'], w=['xc'])
            for k in range(1, 4):
                S.op('dve', lambda e, j=j, k=k: e.scalar_tensor_tensor(
                    xc3, pb[:, :, k:k + L], K.pk[:, cwo + j * 4 + k:cwo + j * 4 + k + 1], xc3, ALU.mult, ALU.add),
                    r=['pb', 'pk', 'xc'], w=['xc'])
            S.op('dve', lambda e: e.tensor_copy(xcb, xc), r=['xc'], w=['xcb'])
            for d in range(2):
                for b in range(nb):
                    S.op('pe', lambda e, d=d, j=j, b=b: e.matmul(ps[4 + b], wr[:, d, j, :], xcb[:, b * 512:(b + 1) * 512],
                                                                  start=True, stop=True), r=['wr', 'xcb'], w=['ps%d' % (4 + b)])
                    S.op('pe', lambda e, d=d, j=j, b=b: e.matmul(ps[6 + b], wi[:, d, j, :], xcb[:, b * 512:(b + 1) * 512],
                                                                  start=True, stop=True), r=['wi', 'xcb'], w=['ps%d' % (6 + b)])
                    bs = slice(b * 512, (b + 1) * 512)
                    S.op('act', lambda e, d=d, j=j, b=b, bs=bs: e.activation(
                        t1[:, bs], ps[4 + b], AF.Sigmoid, bias=K.pkc('b_r', d * 8 + j)), r=['ps%d' % (4 + b), 'pk'], w=['t1'])
                    S.op('act', lambda e, d=d, j=j, b=b, bs=bs: e.activation(
                        t2[:, bs], ps[6 + b], AF.Sigmoid, bias=K.pkc('b_i', d * 8 + j)), r=['ps%d' % (6 + b), 'pk'], w=['t2'])
                S.op('act', lambda e, d=d, j=j: e.activation(aa, t1, AF.Exp, scale=K.c8[:, d * 8 + j:d * 8 + j + 1]),
                     r=['t1', 'c8'], w=['aa'])
                S.op('dve', lambda e: e.tensor_tensor(t1, aa, aa, ALU.mult), r=['aa'], w=['t1'])
                S.op('act', lambda e: e.activation(t1, t1, AF.Sqrt, scale=-1.0, bias=1.0), r=['t1'], w=['t1'])
                S.op('dve', lambda e: e.tensor_tensor(t2, t2, xc, ALU.mult), r=['t2', 'xc'], w=['t2'])
                S.op('dve', lambda e: e.tensor_tensor(bb, t1, t2, ALU.mult), r=['t1', 't2'], w=['bb'])
                for seg in range(nseg):
                    sl = slice(seg * L, (seg + 1) * L)
                    init = 0.0 if grp == 'P' else K.pkc('h0', d * 8 + j)
                    if d == 0:
                        S.op('dve', lambda e, sl=sl, init=init: e.tensor_tensor_scan(hh[0][:, sl], aa[:, sl], bb[:, sl], init,
                                                                                     ALU.mult, ALU.add),
                             r=['aa', 'bb', 'pk'], w=['hh0'])
                    else:
                        S.op('dve', lambda e, sl=sl, init=init: e.tensor_tensor_scan(
                            hh[1][:, sl][:, ::-1], aa[:, sl][:, ::-1], bb[:, sl][:, ::-1], init, ALU.mult, ALU.add),
                            r=['aa', 'bb', 'pk'], w=['hh1'])
                    if grp == 'P':
                        hc = seg * 16 + d * 8 + j
                        pos = seg * L + (L - 1 if d == 0 else 0)
                        S.op('act', lambda e, hc=hc, pos=pos, d=d: e.activation(K.hst[:, hc:hc + 1], hh[d][:, pos:pos + 1], AF.Copy),
                             r=['hh%d' % d], w=['hst'])
            S.op('act', lambda e: e.activation(gbf, gbf, AF.Gelu_apprx_tanh), r=['gbf'], w=['gbf'])
            S.op('dve', lambda e: e.tensor_tensor(hh[0], hh[0], hh[1], ALU.add), r=['hh0', 'hh1'], w=['hh0'])
            S.op('dve', lambda e, j=j: e.tensor_tensor(catT[:, 8 + j, :], hh[0], gbf, ALU.mult), r=['hh0', 'gbf'], w=['catT'])
    if grp == 'P':
        S.op('sp', lambda e: [e.dma_start(out=O['hout'], in_=K.hst)], r=['hst'], chan='st_h')
    S.barrier()
    for t in range(ncols // 512):
        A.off = base
        xt_pre = A.at(uT_off, (NCH, 512)) if grp == 'S' else None
        cnd = [(0, 512, cond)]
        wout_postnorm(K, 0, I['l0_w_out'], catT, 'catT', t * 512, 512, cnd, _ColShift(I['xT'], col0), _ColShift(K.XS1, col0),
                      ws, 2, 0, 'm0', xt=xt_pre)


class _ColShift:
    def __init__(self, ap, off):
        self.ap, self.off = ap, off

    def __getitem__(self, idx):
        p, c, s = idx
        return self.ap[p, c, slice(s.start + self.off, s.stop + self.off)]


def ffn_pass(K, uT, ukey, halves, w1_ap, w3_ap, w2_ap, dff, acc, acckey, ws13, ws2, hbuf, sbuf, first_write):
    S, ps = K.S, K.ps
    nun = dff // 256
    assert nun % 2 == 0
    it = 0

    def load(g):
        return (ws13.load(K.wunit(w1_ap, g * 256, 256)), ws13.load(K.wunit(w3_ap, g * 256, 256)),
                ws2.load(w2_ap[g * 256:(g + 1) * 256, :].rearrange("(m p) n -> p m n", p=128)))
    L = {0: load(0)}
    for g in range(nun):
        (s1, k1), (s3, k3), _ = L[g]
        pr = g // 2
        hb, hk = hbuf[pr % 2], 'hb%d' % (pr % 2)
        for m in range(2):
            mm_ = (g % 2) * 2 + m
            for (c0, n) in halves:
                b = it % 2
                it += 1
                K.mm_group(ps[b][:, 0:n], [s1[:, k, m * 128:(m + 1) * 128] for k in range(NCH)],
                           [uT[:, k, c0:c0 + n] for k in range(NCH)], r=[k1, ukey], w=['ps%d' % b])
                K.mm_group(ps[2 + b][:, 0:n], [s3[:, k, m * 128:(m + 1) * 128] for k in range(NCH)],
                           [uT[:, k, c0:c0 + n] for k in range(NCH)], r=[k3, ukey], w=['ps%d' % (2 + b)])
                sb_, sk = sbuf[b], 'sb%d' % b
                S.op('act', lambda e, b=b, n=n, sb_=sb_: e.activation(sb_[:, 0:n], ps[b][:, 0:n], AF.Silu),
                     r=['ps%d' % b], w=[sk])
                S.op('dve', lambda e, b=b, n=n, sb_=sb_, hb=hb, mm_=mm_, c0=c0: e.tensor_tensor(
                    hb[:, mm_, c0:c0 + n], ps[2 + b][:, 0:n], sb_[:, 0:n], ALU.mult), r=['ps%d' % (2 + b), sk], w=[hk])
        if g + 1 < nun:
            L[g + 1] = load(g + 1)
        if g % 2 == 0:
            continue
        (sa, ka), (sb2, kb2) = L[g - 1][2], L[g][2]
        for d in range(NCH):
            for (c0, n) in halves:
                b = it % 2
                it += 1
                K.mm_group(ps[4 + b][:, 0:n],
                           [sa[:, m, d * 128:(d + 1) * 128] for m in range(2)] + [sb2[:, m, d * 128:(d + 1) * 128] for m in range(2)],
                           [hb[:, m, c0:c0 + n] for m in range(4)], r=[ka, kb2, hk], w=['ps%d' % (4 + b)])
                ak = '%s_%d' % (acckey, d)
                if pr == 0 and first_write:
                    S.op('act', lambda e, b=b, n=n, d=d, c0=c0: e.activation(acc[:, d, c0:c0 + n], ps[4 + b][:, 0:n], AF.Copy),
                         r=['ps%d' % (4 + b)], w=[ak])
                else:
                    S.op('dve', lambda e, b=b, n=n, d=d, c0=c0: e.tensor_tensor(
                        acc[:, d, c0:c0 + n], acc[:, d, c0:c0 + n], ps[4 + b][:, 0:n], ALU.add),
                        r=['ps%d' % (4 + b), ak], w=[ak])
        del L[g - 1]


def phase_ffn0(K, t):
    S, A, I = K.S, K.A, K.I
    col0 = t * 512
    cond = 0 if t == 0 else 1
    xt = A.alloc((NCH, 512))
    uT = A.alloc((NCH, 512), BF16)
    acc = A.alloc((NCH, 512))
    hbuf = [A.alloc((4, 512), BF16) for _ in range(2)]
    sbuf = [A.alloc(512) for _ in range(2)]
    ws13 = K.WS('wf13', 4, (NCH, 256))
    ws2 = K.WS('wf2', 3, (2, D))
    load_x_tile(K, xt, K.XS1, col0, 512, 'xt', 'ld_x_f0')
    for c in range(NCH):
        S.op('dve', lambda e, c=c: e.tensor_scalar(uT[:, c, :], xt[:, c, :], K.modc(0, 4, c, cond), K.modc(0, 3, c, cond),
                                                   ALU.mult, ALU.add), r=['xt', 'mod'], w=['uT'])
    ffn_pass(K, uT, 'uT', [(0, 512)], I['l0_ffn_w1'], I['l0_ffn_w3'], I['l0_ffn_w2'], DFF, acc, 'acc', ws13, ws2, hbuf, sbuf, True)
    post_norm(K, 0, 5, 1, xt, 'xt', lambda m: (acc[:, m, :], 'acc_%d' % m), [(0, 512, cond)], 512, A)
    S.op('sp', lambda e: [e.dma_start(out=K.XS2[:, :, col0:col0 + 512], in_=xt)], r=['xt'], chan='st_x_f0')


def finalize(K):
    nc, S = K.nc, K.S
    S.finish()
    names = [('e', e) for e in Sched.ENG] + [('c', c) for c in S.chan_cnt]
    st = K.st
    sems = {k: st.enter_context(nc.semaphore(("s_%s_%s" % k).replace('#', '_'))) for k in names}
    block = st.enter_context(nc.Block())

    @block.tensor
    def _(e):
        S.emit('pe', e, sems)

    @block.scalar
    def _(e):
        S.emit('act', e, sems)

    @block.vector
    def _(e):
        S.emit('dve', e, sems)

    @block.gpsimd
    def _(e):
        S.emit('pool', e, sems)

    @block.sync
    def _(e):
        S.emit('sp', e, sems)
    st.close()
    return nc


STAGE = 99


def build_all(debug=False, stage=None):
    stage = STAGE if stage is None else stage
    K = build_program(debug)

    def sep():
        K.S.barrier()
        K.A.reset()
    if stage >= 1:
        phase_mixer0(K, 'P')
        sep()
    if stage >= 2:
        phase_mixer0(K, 'S')
        sep()
    if stage >= 3:
        for t in range(3):
            phase_ffn0(K, t)
            sep()
    if stage >= 4:
        phase_filter(K, 256)
        sep()
        phase_filter(K, 1024)
        sep()
    if stage >= 5:
        phase_hyena(K, 'P')
        sep()
        phase_hyena(K, 'S')
        sep()
    if stage >= 6:
        phase_moe(K)
        sep()
        phase_final(K)
        sep()
    return finalize(K)


def _dft_consts(L):
    t = np.arange(L, dtype=np.float64)
    th = np.pi * np.outer(t, t) / L
    sgn = np.where(np.arange(L) % 2 == 0, 1.0, -1.0)
    FC = np.cos(th)
    FS = -np.sin(th)
    FS[:, 0] = sgn
    IC = np.cos(th) / L
    IC[0, :] = 1.0 / (2 * L)
    IS = -np.sin(th) / L
    IS[0, :] = sgn / (2 * L)
    NY2 = np.zeros((L, L))
    NY2[:, 0] = 2 * sgn
    return np.stack([FC, FS, IC, IS, NY2]).astype(np.float32)


def _feat(L):
    t = np.arange(L, dtype=np.float32)
    t01 = t / np.float32(L - 1)
    w = np.float32(2.0 * math.pi) * t / np.float32(L)
    f = np.linspace(1e-4, 15, 16, dtype=np.float32)
    fw = w[:, None] * f[None, :]
    feat = np.concatenate([t01[:, None], np.cos(fw), -np.sin(fw)], -1).astype(np.float32)
    return np.ascontiguousarray(feat.T)


def _cols(v, n):
    return np.ascontiguousarray(np.asarray(v, np.float32).reshape(n, 128).T)


def make_in_maps(inp):
    f = lambda a: np.ascontiguousarray(np.asarray(a, np.float32))
    shared = {}
    def tile_w(W, ncols):
        W = np.asarray(W, np.float32)
        N = W.shape[1]
        return np.ascontiguousarray(W.reshape(16, 128, N // ncols, ncols).transpose(2, 1, 0, 3))
    for nm, nc_ in [('l0_ada_w', 512), ('l0_w_in', 512), ('l0_w_out', 512), ('l0_ffn_w1', 256), ('l0_ffn_w3', 256),
                    ('l1_ada_w', 512), ('l1_w_in', 128), ('l1_w_out', 512)]:
        shared[nm] = tile_w(inp[nm], nc_)
    shared['l0_ffn_w2'] = f(inp['l0_ffn_w2'])
    shared['exp_w1'] = np.stack([tile_w(inp['l1_exp_w1'][e_], 256) for e_ in range(NEXP)])
    shared['exp_w3'] = np.stack([tile_w(inp['l1_exp_w3'][e_], 256) for e_ in range(NEXP)])
    shared['exp_w2'] = f(inp['l1_exp_w2'])
    shared['lru_wr'] = f(np.transpose(inp['l0_lru_w_r'], (2, 0, 1, 3)))
    shared['lru_wi'] = f(np.transpose(inp['l0_lru_w_i'], (2, 0, 1, 3)))
    shared['router'] = f(np.asarray(inp['l1_router']).reshape(16, 128, 8).transpose(1, 0, 2))
    shared['qg'] = f(np.broadcast_to(np.asarray(inp['l0_q_norm'])[None, :], (128, 128)))
    shared['kg'] = f(np.broadcast_to(np.asarray(inp['l0_k_norm'])[None, :], (128, 128)))
    tt = np.arange(1024)
    inv = 1.0 / (10000.0 ** (np.arange(32, dtype=np.float32) / 32))
    ang = np.concatenate([(tt // 64).astype(np.float32)[:, None] * inv, (tt % 64).astype(np.float32)[:, None] * inv], -1)
    rope = np.stack([np.cos(ang), np.sin(ang)], 1).astype(np.float32)
    shared['rope'] = f(rope.reshape(8, 128, 2, 64).transpose(1, 0, 2, 3))
    shared['featT256'] = _feat(256)
    shared['featT1024'] = _feat(1024)
    shared['fw1'] = f(inp['l1_filt_w1'])
    shared['fw2'] = f(inp['l1_filt_w2'])
    shared['fw3'] = f(inp['l1_filt_w3'])
    shared['pk64'] = f(np.stack([inp['l1_filt_b1'], inp['l1_filt_f1'], inp['l1_filt_b2'], inp['l1_filt_f2']], 1))
    shared['decay'] = f(np.asarray(inp['l1_filt_decay']).reshape(1, 8192))
    for L in (256, 1024):
        shared['nt01_%d' % L] = f((-(np.arange(L, dtype=np.float32) / np.float32(L - 1))).reshape(L // 128, 128).T)
        shared['dft%d' % L] = _dft_consts(L)
    shared['ident'] = np.eye(128, dtype=np.float32)
    sel = np.zeros((8, 8, 128), np.float32)
    for e_ in range(8):
        sel[e_, e_, :] = 1.0
    shared['sel'] = sel.reshape(8, 1024)
    shared['tri'] = np.triu(np.ones((128, 128), np.float32), 1)
    shared['iota'] = np.ascontiguousarray(np.broadcast_to(np.arange(512, dtype=np.float32)[None, :], (128, 512)))
    pk_common = np.zeros((128, NPK), np.float32)

    def put(name, arr):
        o, w = PK[name]
        assert arr.shape == (128, w), (name, arr.shape)
        pk_common[:, o:o + w] = arr
    put('ada_b0', _cols(inp['l0_ada_b'], 96))
    put('ada_b1', _cols(inp['l1_ada_b'], 96))
    put('ln', np.concatenate([_cols(inp[n], 16) for n in ['l0_ln1_g', 'l0_ln1_b', 'l0_ln2_g', 'l0_ln2_b',
                                                         'l1_ln1_g', 'l1_ln1_b', 'l1_ln2_g', 'l1_ln2_b']], 1))
    put('conv_w', f(np.asarray(inp['l0_lru_conv_w']).reshape(4, 8, 128).transpose(2, 1, 0).reshape(128, 32)))
    put('conv_b', _cols(inp['l0_lru_conv_b'], 8))
    put('lam', f(np.asarray(inp['l0_lru_lambda']).reshape(2, 8, 128).transpose(2, 0, 1).reshape(128, 16)))
    put('b_r', f(np.asarray(inp['l0_lru_b_r']).reshape(2, 8, 128).transpose(2, 0, 1).reshape(128, 16)))
    put('b_i', f(np.asarray(inp['l0_lru_b_i']).reshape(2, 8, 128).transpose(2, 0, 1).reshape(128, 16)))
    put('short_w', f(np.asarray(inp['l1_short_w']).reshape(3, 48, 128).transpose(2, 1, 0).reshape(128, 144)))
    put('short_b', _cols(inp['l1_short_b'], 48))
    put('fbias', f(np.asarray(inp['l1_filt_bias']).reshape(2, 16, 128).transpose(2, 0, 1).reshape(128, 32)))
    xp = np.asarray(inp['x_prompt'], np.float32)
    xs = np.asarray(inp['x_sample'], np.float32)
    maps = []
    for i in range(8):
        s, q = i // 4, i % 4
        m = dict(shared)
        X = np.concatenate([xp[2 * i], xp[2 * i + 1], xs[s]], 0)
        m['xT'] = f(X.T.reshape(16, 128, NCOL).transpose(1, 0, 2))
        cond = np.stack([np.asarray(inp['c_ctx'], np.float32), np.asarray(inp['c'], np.float32)[s]], 0)
        m['condT'] = f(cond.T.reshape(16, 128, 2).transpose(1, 0, 2))
        pkk = pk_common.copy()
        o, w = PK['h0']
        pkk[:, o:o + w] = np.asarray(inp['state_l0_lru'], np.float32)[s].reshape(2, 8, 128).transpose(2, 0, 1).reshape(128, 16)
        o, w = PK['qmask']
        pkk[:, o:o + w] = 0.0
        pkk[:, o + q] = 1.0
        m['pk'] = pkk
        m['ckT'] = f(np.asarray(inp['cache_l0_k'], np.float32)[s].transpose(2, 1, 0))
        m['cv'] = f(np.asarray(inp['cache_l0_v'], np.float32)[s].reshape(2, 128, 256).transpose(1, 0, 2))
        maps.append(m)
    return maps


_PROG = {}


def kernel(**inputs):
    maps = make_in_maps(inputs)
    if 'nc' not in _PROG:
        _PROG['nc'] = build_all()
    res = run_bass_kernel_spmd(_PROG['nc'], maps, core_ids=list(range(8)))
    y_prompt = np.zeros((16, 256, D), np.float32)
    y_sample = np.zeros((2, 1024, D), np.float32)
    nk = np.zeros((16, 256, 2, 128), np.float32)
    nv = np.zeros((16, 256, 2, 128), np.float32)
    nh = np.zeros((16, 2, 1024), np.float32)
    for i in range(8):
        r = res.results[i]
        yT = np.asarray(r['yT'])
        Y = yT.transpose(2, 1, 0).reshape(NMOE, D)
        y_prompt[2 * i] = Y[0:256]
        y_prompt[2 * i + 1] = Y[256:512]
        s, q = i // 4, i % 4
        y_sample[s, q * 256:(q + 1) * 256] = Y[512:768]
        ko = np.asarray(r['kout']).reshape(2, 256, 2, 128)
        vo = np.asarray(r['vout']).reshape(2, 256, 2, 128)
        nk[2 * i], nk[2 * i + 1] = ko[0], ko[1]
        nv[2 * i], nv[2 * i + 1] = vo[0], vo[1]
        ho = np.asarray(r['hout']).reshape(128, 2, 2, 8)
        hh = ho.transpose(1, 2, 3, 0).reshape(2, 2, 1024)
        nh[2 * i], nh[2 * i + 1] = hh[0], hh[1]
    return (y_prompt, y_sample, nk, nv, nh)


def phase_filter(K, L):
    S, A, I, ps = K.S, K.A, K.I, K.ps
    nt = L // 128
    KFL = K.KF[L]
    featb = A.alloc(L, BF16)
    fw1b = A.alloc(64, BF16)
    fw2b = A.alloc(64, BF16)
    fw3b = A.alloc(8192, BF16)
    pk64 = A.alloc(4)
    absd = A.alloc(8192)
    nt01 = A.alloc(nt)
    FCb = A.alloc((nt, L), BF16)
    FSb = A.alloc((nt, L), BF16)
    NYb = A.alloc((nt, 128), BF16)
    arg = A.alloc(512)
    rr = A.alloc(512)
    h1b = A.alloc(L, BF16)
    h2b = A.alloc(L, BF16)
    E = A.alloc((2, 512))
    hdec = A.alloc((2, 512))
    hs = A.alloc((nt, 512), BF16)
    hd = A.alloc((nt, 512), BF16)
    h1p = A.alloc((nt, 512), BF16)
    stg = [A.alloc(512) for _ in range(6)]
    dft = I['dft%d' % L]
    S.op('pool', lambda e: [e.dma_start(out=featb[0:33, :], in_=I['featT%d' % L])], w=['featb'], chan='f_feat')
    S.op('pool', lambda e: [e.dma_start(out=fw1b[0:33, :], in_=I['fw1'])], w=['fw1b'], chan='f_w1')
    S.op('pool', lambda e: [e.dma_start(out=fw2b[0:64, :], in_=I['fw2'])], w=['fw2b'], chan='f_w2')
    S.op('pool', lambda e: [e.dma_start(out=fw3b[0:64, :], in_=I['fw3'], max_dma_last_dim=8192)], w=['fw3b'], chan='f_w3')
    S.op('sp', lambda e: [e.dma_start(out=pk64[0:64, :], in_=I['pk64'])], w=['pk64'], chan='f_pk64')
    S.op('sp', lambda e: [e.dma_start(out=absd, in_=I['decay'].partition_broadcast(128))], w=['absd'], chan='f_dec')
    S.op('sp', lambda e: [e.dma_start(out=nt01, in_=I['nt01_%d' % L])], w=['nt01'], chan='f_nt01')
    S.op('pool', lambda e: [e.dma_start(out=FCb, in_=dft[0].rearrange("(a p) f -> p a f", p=128))], w=['FCb'], chan='f_fc')
    S.op('pool', lambda e: [e.dma_start(out=FSb, in_=dft[1].rearrange("(a p) f -> p a f", p=128))], w=['FSb'], chan='f_fs')
    S.op('pool', lambda e: [e.dma_start(out=NYb, in_=dft[4][:, 0:128].rearrange("(a p) f -> p a f", p=128))], w=['NYb'], chan='f_ny')
    S.op('act', lambda e: e.activation(absd, absd, AF.Abs), r=['absd'], w=['absd'])

    def sin_layer(lhsT, lkey, kp, rhsb, rkey, bcol, fcol, outb, okey):
        for b in range(L // min(L, 512)):
            n = min(L, 512)
            cs = slice(b * n, (b + 1) * n)
            S.op('pe', lambda e, cs=cs, n=n: e.matmul(ps[0][0:64, 0:n], lhsT[0:kp, 0:64], rhsb[0:kp, cs], start=True, stop=True),
                 r=[lkey, rkey], w=['ps0'])
            S.op('dve', lambda e, n=n: e.tensor_scalar(arg[0:64, 0:n], ps[0][0:64, 0:n], pk64[0:64, bcol:bcol + 1],
                                                       pk64[0:64, fcol:fcol + 1], ALU.add, ALU.mult), r=['ps0', 'pk64'], w=['arg'])
            S.op('dve', lambda e, n=n: e.tensor_scalar(rr[0:64, 0:n], arg[0:64, 0:n], 1.0 / (2 * math.pi), MAGIC, ALU.mult, ALU.add),
                 r=['arg'], w=['rr'])
            S.op('dve', lambda e, n=n: e.tensor_scalar(rr[0:64, 0:n], rr[0:64, 0:n], MAGIC, -2 * math.pi, ALU.subtract, ALU.mult),
                 r=['rr'], w=['rr'])
            S.op('dve', lambda e, n=n: e.tensor_tensor(arg[0:64, 0:n], arg[0:64, 0:n], rr[0:64, 0:n], ALU.add), r=['arg', 'rr'], w=['arg'])
            S.op('dve', lambda e, n=n: e.tensor_scalar(arg[0:64, 0:n], arg[0:64, 0:n], 3.1415925, -3.1415925, ALU.min, ALU.max), r=['arg'], w=['arg'])
            S.op('act', lambda e, cs=cs, n=n: e.activation(outb[0:64, cs], arg[0:64, 0:n], AF.Sin), r=['arg'], w=[okey])

    sin_layer(fw1b, 'fw1b', 33, featb, 'featb', 0, 1, h1b, 'h1b')
    sin_layer(fw2b, 'fw2b', 64, h1b, 'h1b', 2, 3, h2b, 'h2b')
    si = 0
    for cu in range(8):
        order, chq = cu // 4, cu % 4
        cbase = order * 2048 + chq * 512
        for tb in range(nt):
            for side in range(2):
                cc = side * 4096 + cbase
                S.op('pe', lambda e, side=side, tb=tb, cc=cc: e.matmul(ps[side], h2b[0:64, tb * 128:(tb + 1) * 128],
                                                                      fw3b[0:64, cc:cc + 512], start=True, stop=True),
                     r=['h2b', 'fw3b'], w=['ps%d' % side])
                S.op('act', lambda e, side=side, tb=tb, cc=cc: e.activation(E[:, side, :], absd[:, cc:cc + 512], AF.Exp,
                                                                            scale=nt01[:, tb:tb + 1]), r=['absd', 'nt01'], w=['E%d' % side])
                S.op('dve', lambda e, side=side: e.tensor_tensor(hdec[:, side, :], ps[side], E[:, side, :], ALU.mult),
                     r=['ps%d' % side, 'E%d' % side], w=['hdec'])
            if tb == 0:
                S.op('dve', lambda e: e.memset(hdec[0:1, 1, :], 0.0), r=['hdec'], w=['hdec'])
            S.op('dve', lambda e, tb=tb: e.tensor_tensor(hs[:, tb, :], hdec[:, 0, :], hdec[:, 1, :], ALU.add), r=['hdec'], w=['hs'])
            S.op('dve', lambda e, tb=tb: e.tensor_tensor(hd[:, tb, :], hdec[:, 0, :], hdec[:, 1, :], ALU.subtract), r=['hdec'], w=['hd'])
            S.op('act', lambda e, tb=tb: e.activation(h1p[:, tb, :], hdec[:, 1, :], AF.Copy), r=['hdec'], w=['h1p'])
        for fc in range(nt):
            fs_ = slice(fc * 128, (fc + 1) * 128)
            K.mm_group(ps[2], [FCb[:, tb, fs_] for tb in range(nt)], [hs[:, tb, :] for tb in range(nt)], r=['FCb', 'hs'], w=['ps2'])
            lh = [FSb[:, tb, fs_] for tb in range(nt)]
            rh = [hd[:, tb, :] for tb in range(nt)]
            if fc == 0:
                lh += [NYb[:, tb, :] for tb in range(nt)]
                rh += [h1p[:, tb, :] for tb in range(nt)]
            K.mm_group(ps[3], lh, rh, r=['FSb', 'NYb', 'hd', 'h1p'], w=['ps3'])
            a_, b_, d_ = stg[si % 2 * 3], stg[si % 2 * 3 + 1], stg[si % 2 * 3 + 2]
            ka, kb, kd = 'stgA%d' % (si % 2), 'stgB%d' % (si % 2), 'stgD%d' % (si % 2)
            si += 1
            S.op('act', lambda e, a_=a_: e.activation(a_, ps[2], AF.Copy), r=['ps2'], w=[ka])
            S.op('dve', lambda e, b_=b_: e.tensor_copy(b_, ps[3]), r=['ps3'], w=[kb])
            dsl = slice(cbase, cbase + 512)
            S.op('sp', lambda e, a_=a_, fc=fc, dsl=dsl: [e.dma_start(out=KFL[0, fc, :, dsl], in_=a_)], r=[ka], w=['KF'], chan=ka)
            if fc == 0:
                S.op('act', lambda e, a_=a_, d_=d_: e.activation(d_, a_, AF.Copy), r=[ka], w=[kd])
                S.op('act', lambda e, b_=b_, d_=d_: e.activation(d_[0:1, :], b_[0:1, :], AF.Copy), r=[kb, kd], w=[kd])
                S.op('dve', lambda e, b_=b_: e.memset(b_[0:1, :], 0.0), r=[kb, kd], w=[kb])
                S.op('sp', lambda e, d_=d_, dsl=dsl: [e.dma_start(out=KFL[2, 0, :, dsl], in_=d_)], r=[kd], w=['KF'], chan=kd)
            S.op('sp', lambda e, b_=b_, fc=fc, dsl=dsl: [e.dma_start(out=KFL[1, fc, :, dsl], in_=b_)], r=[kb], w=['KF'], chan=kb)


def phase_hyena(K, grp):
    S, A, I, ps, psb = K.S, K.A, K.I, K.ps, K.psb
    if grp == 'P':
        col0, ncols, nseg, L, cond = 0, 512, 2, 256, 0
    else:
        col0, ncols, nseg, L, cond = 512, 1024, 1, 1024, 1
    nt = L // 128
    nb = ncols // 512
    KFL = K.KF[L]
    dft = I['dft%d' % L]
    uT = A.alloc((NCH, ncols), BF16)
    base = A.off
    xtmp = A.alloc((NCH, 512))
    for t in range(nb):
        load_x_tile(K, xtmp, K.XS2, col0 + t * 512, 512, 'xtmp', 'ld_xtmp_h')
        for c in range(NCH):
            S.op('dve', lambda e, c=c, t=t: e.tensor_scalar(
                uT[:, c, t * 512:(t + 1) * 512], xtmp[:, c, :], K.modc(1, 1, c, cond), K.modc(1, 0, c, cond),
                ALU.mult, ALU.add), r=['xtmp', 'mod'], w=['uT'])
    S.barrier()
    A.off = base
    M = [A.alloc((nt, L), BF16) for _ in range(4)]
    for i in range(4):
        S.op('pool', lambda e, i=i: [e.dma_start(out=M[i], in_=dft[i].rearrange("(a p) f -> p a f", p=128))],
             w=['M%d' % i], chan='h_m%d' % i)
    ws = K.WS('wh', 3, (NCH, 128))
    pbs = [A.alloc((nseg, L + 2)) for _ in range(3)]
    cx = [A.alloc(ncols) for _ in range(3)]
    zcur = A.alloc(ncols)
    srcb = A.alloc(ncols, BF16)
    zt = A.alloc((nt, 128), BF16)
    KA = [A.alloc((nt, 128)) for _ in range(2)]
    KB = [A.alloc((nt, 128)) for _ in range(2)]
    KD = [A.alloc((nt, 128)) for _ in range(2)]
    Pre = A.alloc((nt, 128), BF16)
    Pim = A.alloc((nt, 128), BF16)
    t1 = A.alloc(512)
    t2 = A.alloc(512)
    tmp = A.alloc(ncols)
    for i in range(3):
        S.op('dve', lambda e, i=i: e.memset(pbs[i], 0.0), w=['pb%d' % i])
    swo, sbo, fbo = PK['short_w'][0], PK['short_b'][0], PK['fbias'][0]
    seg3 = lambda ap: ap.rearrange("p (s l) -> p s l", l=L)
    NQ = min(L, 512)
    kit = 0
    for c in range(NCH):
        for wi_ in range(3):
            slot, wkey = ws.load(K.wunit(I['l1_w_in'], wi_ * 2048 + c * 128, 128))
            eidx = wi_ * 16 + c
            for b in range(nb):
                K.mm_group(ps[b], [slot[:, k, :] for k in range(NCH)], [uT[:, k, b * 512:(b + 1) * 512] for k in range(NCH)],
                           r=[wkey, 'uT'], w=['ps%d' % b])
                if grp == 'P':
                    S.op('act', lambda e, b=b, wi_=wi_: e.activation(pbs[wi_][:, :, 1:L + 1], seg3(ps[b]), AF.Copy),
                         r=['ps%d' % b], w=['pb%d' % wi_])
                else:
                    S.op('act', lambda e, b=b, wi_=wi_: e.activation(pbs[wi_][:, 0, 1 + b * 512:1 + (b + 1) * 512], ps[b], AF.Copy),
                         r=['ps%d' % b], w=['pb%d' % wi_])
            o3 = seg3(cx[wi_])
            S.op('dve', lambda e, wi_=wi_, eidx=eidx, o3=o3: e.tensor_scalar(
                o3, pbs[wi_][:, :, 0:L], K.pk[:, swo + eidx * 3:swo + eidx * 3 + 1], K.pk[:, sbo + eidx:sbo + eidx + 1],
                ALU.mult, ALU.add), r=['pb%d' % wi_, 'pk'], w=['cx%d' % wi_])
            for k in (1, 2):
                S.op('dve', lambda e, wi_=wi_, eidx=eidx, o3=o3, k=k: e.scalar_tensor_tensor(
                    o3, pbs[wi_][:, :, k:k + L], K.pk[:, swo + eidx * 3 + k:swo + eidx * 3 + k + 1], o3, ALU.mult, ALU.add),
                    r=['pb%d' % wi_, 'pk', 'cx%d' % wi_], w=['cx%d' % wi_])
        for n in range(2):
            src, skey = (cx[2], 'cx2') if n == 0 else (zcur, 'zcur')
            gate, gkey = (cx[0], 'cx0') if n == 0 else (cx[1], 'cx1')
            kk = 0
            csl = slice(n * 2048 + c * 128, n * 2048 + (c + 1) * 128)
            S.op('sp', lambda e, kk=kk, csl=csl: [e.dma_start(out=KA[kk], in_=KFL[0, :, :, csl].rearrange("a p f -> p a f"))],
                 r=['KF'], w=['KA%d' % kk], chan='h_ka%d' % kk)
            S.op('sp', lambda e, kk=kk, csl=csl: [e.dma_start(out=KB[kk], in_=KFL[1, 0:nt, :, csl].rearrange("a p f -> p a f"))],
                 r=['KF'], w=['KB%d' % kk], chan='h_kb%d' % kk)
            S.op('sp', lambda e, kk=kk, csl=csl: [e.dma_start(out=KD[kk][:, 1:nt, :], in_=KFL[0, 1:nt, :, csl].rearrange("a p f -> p a f"))],
                 r=['KF'], w=['KD%d' % kk], chan='h_kd%d' % kk)
            S.op('sp', lambda e, kk=kk, csl=csl: [e.dma_start(out=KD[kk][:, 0, :], in_=KFL[2, 0, :, csl])],
                 r=['KF'], w=['KD%d' % kk], chan='h_kd%d' % kk)
            S.op('act', lambda e, src=src: e.activation(srcb, src, AF.Copy), r=[skey], w=['srcb'])
            for seg in range(nseg):
                def fn(e, seg=seg):
                    ins = None
                    for tb in range(nt):
                        ins = e.transpose(psb[6][:, tb * 128:(tb + 1) * 128], srcb[:, seg * L + tb * 128:seg * L + (tb + 1) * 128], K.ident)
                    return ins
                S.op('pe', fn, r=['srcb', 'ident'], w=['ps6'])
                S.op('act', lambda e: e.activation(zt, psb[6][:, 0:nt * 128].rearrange("p (a t) -> p a t", t=128), AF.Copy),
                     r=['ps6'], w=['zt'])
                for fc in range(nt):
                    bq, off = fc // 4, (fc % 4) * 128
                    fs_ = slice(fc * 128, (fc + 1) * 128)
                    K.mm_group(ps[0 + bq][:, off:off + 128], [M[0][:, tb, fs_] for tb in range(nt)], [zt[:, tb, :] for tb in range(nt)],
                               r=['M0', 'zt'], w=['ps%d' % bq])
                    K.mm_group(ps[2 + bq][:, off:off + 128], [M[1][:, tb, fs_] for tb in range(nt)], [zt[:, tb, :] for tb in range(nt)],
                               r=['M1', 'zt'], w=['ps%d' % (2 + bq)])
                for bq in range((nt + 3) // 4):
                    nf = min(4, nt - bq * 4)
                    w_ = nf * 128
                    fsl = slice(bq * 4, bq * 4 + nf)
                    f2 = lambda ap, fsl=fsl: ap[:, fsl, :]
                    v3 = lambda ap, w_=w_: ap[:, 0:w_].rearrange("p (a f) -> p a f", f=128)
                    ur, ui = ps[bq], ps[2 + bq]
                    ukr, uki = 'ps%d' % bq, 'ps%d' % (2 + bq)
                    S.op('dve', lambda e, ur=ur, f2=f2, v3=v3, kk=kk: e.tensor_tensor(v3(t1), v3(ur), f2(KA[kk]), ALU.mult), r=[ukr, 'KA%d' % kk], w=['t1'])
                    S.op('dve', lambda e, ui=ui, f2=f2, v3=v3, kk=kk: e.tensor_tensor(v3(t2), v3(ui), f2(KB[kk]), ALU.mult), r=[uki, 'KB%d' % kk], w=['t2'])
                    S.op('dve', lambda e, f2=f2, v3=v3: e.tensor_tensor(f2(Pre), v3(t1), v3(t2), ALU.subtract), r=['t1', 't2'], w=['Pre'])
                    S.op('dve', lambda e, ur=ur, f2=f2, v3=v3, kk=kk: e.tensor_tensor(v3(t1), v3(ur), f2(KB[kk]), ALU.mult), r=[ukr, 'KB%d' % kk, 'Pre'], w=['t1'])
                    S.op('dve', lambda e, ui=ui, f2=f2, v3=v3, kk=kk: e.tensor_tensor(v3(t2), v3(ui), f2(KD[kk]), ALU.mult), r=[uki, 'KD%d' % kk, 'Pre'], w=['t2'])
                    S.op('dve', lambda e, f2=f2, v3=v3: e.tensor_tensor(f2(Pim), v3(t1), v3(t2), ALU.add), r=['t1', 't2'], w=['Pim'])
                for hq in range(L // NQ):
                    tsl = slice(hq * NQ, (hq + 1) * NQ)
                    yb_ = ps[4 + hq % 2]
                    yk = 'ps%d' % (4 + hq % 2)
                    K.mm_group(yb_[:, 0:NQ], [Pre[:, fc, :] for fc in range(nt)] + [Pim[:, fc, :] for fc in range(nt)],
                               [M[2][:, fc, tsl] for fc in range(nt)] + [M[3][:, fc, tsl] for fc in range(nt)],
                               r=['Pre', 'Pim', 'M2', 'M3'], w=[yk])
                    gs = slice(seg * L + hq * NQ, seg * L + (hq + 1) * NQ)
                    S.op('dve', lambda e, gs=gs, yb_=yb_, src=src, n=n, c=c: e.scalar_tensor_tensor(
                        tmp[:, gs], src[:, gs], K.pk[:, fbo + n * 16 + c:fbo + n * 16 + c + 1], yb_[:, 0:NQ], ALU.mult, ALU.add),
                        r=[yk, skey, 'pk'], w=['tmp'])
            S.op('dve', lambda e, gate=gate: e.tensor_tensor(zcur, tmp, gate, ALU.mult), r=['tmp', gkey, 'srcb', skey], w=['zcur'])
        S.op('sp', lambda e, c=c: [e.dma_start(out=K.ZZ[:, c, col0:col0 + ncols], in_=zcur)], r=['zcur'], w=['ZZ'], chan='st_zz')
    S.barrier()
    A.off = base
    zzT = A.alloc((NCH, 512), BF16)
    ws2 = K.WS('who', 2, (NCH, 512))
    mark = A.off
    for t in range(nb):
        A.off = mark
        S.op('pool', lambda e, t=t: [e.dma_start(out=zzT, in_=K.ZZ[:, :, col0 + t * 512:col0 + (t + 1) * 512])],
             r=['ZZ'], w=['zzT'], chan='ld_zz')
        wout_postnorm(K, 1, I['l1_w_out'], zzT, 'zzT', 0, 512, [(0, 512, cond)], _ColShift(K.XS2, col0 + t * 512),
                      _ColShift(K.XS3, col0 + t * 512), ws2, 2, 2, 'h1')


def load_piece(K, dst, src, piece, tmpq, key):
    S = K.S
    if piece < 2:
        S.op('sp', lambda e: [e.dma_start(out=dst, in_=src[:, :, piece * 256:(piece + 1) * 256])], w=[key], chan='ld_pc')
        return
    qo = PK['qmask'][0]
    for r in range(4):
        S.op('sp', lambda e, r=r: [e.dma_start(out=tmpq, in_=src[:, :, 512 + r * 256:512 + (r + 1) * 256])], w=['tmpq'], chan='ld_pq')
        if r == 0:
            S.op('dve', lambda e, r=r: e.tensor_scalar(dst, tmpq, K.pk[:, qo + r:qo + r + 1], None, ALU.mult), r=['tmpq', 'pk'], w=[key])
        else:
            S.op('dve', lambda e, r=r: e.scalar_tensor_tensor(dst, tmpq, K.pk[:, qo + r:qo + r + 1], dst, ALU.mult, ALU.add),
                 r=['tmpq', 'pk', key], w=[key])


def phase_final(K):
    S, A, O = K.S, K.A, K.O
    xq = A.alloc((NCH, 256))
    tmpq = A.alloc((NCH, 256))
    mo = A.alloc((NCH, 256))
    mark = A.off
    for piece in range(3):
        A.off = mark
        cond = 0 if piece < 2 else 1
        cs = slice(piece * 256, (piece + 1) * 256)
        load_piece(K, xq, K.XS3, piece, tmpq, 'xt')
        S.op('sp', lambda e, cs=cs: [e.dma_start(out=mo, in_=K.MO[:, :, cs])], r=['MO'], w=['mo'], chan='ld_mo')
        post_norm(K, 1, 5, 3, xq, 'xt', lambda m: (mo[:, m, :], 'mo'), [(0, 256, cond)], 256, A)
        S.op('sp', lambda e, cs=cs: [e.dma_start(out=O['yT'][:, :, cs], in_=xq)], r=['xt'], chan='st_y')


I32 = mybir.dt.int32
CAP = 256
MOE_THR = (257, 385, 513)


def phase_moe(K):
    S, A, I, ps, psb = K.S, K.A, K.I, K.ps, K.psb
    NTB = NMOE // 128
    u_tok = A.alloc((NTB, D), BF16)
    maskf = A.alloc((NTB, 8))
    combf = A.alloc((NTB, 8))
    posf = [A.alloc((NTB, 8)) for _ in range(2)]
    cntf = A.alloc(8)
    cnt_i = A.alloc(8).bitcast(I32)
    iota = A.alloc(512)
    tri = A.alloc(128, BF16)
    base = A.off
    uT = A.alloc((NCH, NMOE), BF16)
    loT = A.alloc((NCH, NMOE), BF16)
    xq = A.alloc((NCH, 256))
    tmpq = A.alloc((NCH, 256))
    rf = A.alloc((NCH, 8))
    rhi = A.alloc((NCH, 8), BF16)
    rlo = A.alloc((NCH, 8), BF16)
    maskb = A.alloc((NTB, 8), BF16)
    ut = A.alloc(256)
    lg = A.alloc(8)
    m8 = A.alloc(8)
    sm = A.alloc(8)
    c2 = A.alloc(8)
    S.op('sp', lambda e: [e.dma_start(out=rf, in_=I['router'])], w=['rf'], chan='ld_rt')
    S.op('sp', lambda e: [e.dma_start(out=iota, in_=I['iota'])], w=['iota'], chan='ld_iota')
    S.op('pool', lambda e: [e.dma_start(out=tri, in_=I['tri'])], w=['tri'], chan='ld_tri')
    S.op('dve', lambda e: e.tensor_copy(rhi, rf), r=['rf'], w=['rhi'])
    S.op('dve', lambda e: e.tensor_tensor(rlo, rf, rhi, ALU.subtract), r=['rf', 'rhi'], w=['rlo'])
    for piece in range(3):
        cond = 0 if piece < 2 else 1
        load_piece(K, xq, K.XS3, piece, tmpq, 'xq')
        cs = slice(piece * 256, (piece + 1) * 256)
        for c in range(NCH):
            S.op('dve', lambda e, c=c, cond=cond: e.tensor_scalar(ut, xq[:, c, :], K.modc(1, 4, c, cond), K.modc(1, 3, c, cond),
                                                                  ALU.mult, ALU.add), r=['xq', 'mod'], w=['ut'])
            S.op('act', lambda e, c=c, cs=cs: e.activation(uT[:, c, cs], ut, AF.Copy), r=['ut'], w=['uT'])
            S.op('dve', lambda e, c=c, cs=cs: e.tensor_tensor(loT[:, c, cs], ut, uT[:, c, cs], ALU.subtract), r=['ut', 'uT'], w=['loT'])
    for tb in range(NTB):
        ts_ = slice(tb * 128, (tb + 1) * 128)
        K.mm_group(ps[0][:, 0:8], [uT[:, k, ts_] for k in range(NCH)] + [loT[:, k, ts_] for k in range(NCH)] + [uT[:, k, ts_] for k in range(NCH)],
                   [rhi[:, k, :] for k in range(NCH)] * 2 + [rlo[:, k, :] for k in range(NCH)], r=['uT', 'loT', 'rhi', 'rlo'], w=['ps0'])
        S.op('act', lambda e: e.activation(lg, ps[0][:, 0:8], AF.Copy), r=['ps0'], w=['lg'])
        S.op('dve', lambda e: e.max(m8, lg), r=['lg'], w=['m8'])
        S.op('dve', lambda e: e.tensor_tensor(sm[:, 0:1], m8[:, 1:2], m8[:, 0:1], ALU.subtract), r=['m8'], w=['sm'])
        S.op('act', lambda e: e.activation(sm[:, 1:2], sm[:, 0:1], AF.Exp), r=['sm'], w=['sm'])
        S.op('dve', lambda e: e.tensor_scalar(sm[:, 2:3], sm[:, 1:2], 1.0, None, ALU.add), r=['sm'], w=['sm'])
        S.op('dve', lambda e: e.reciprocal(sm[:, 3:4], sm[:, 2:3]), r=['sm'], w=['sm'])
        S.op('dve', lambda e: e.tensor_tensor(sm[:, 4:5], sm[:, 1:2], sm[:, 3:4], ALU.mult), r=['sm'], w=['sm'])
        mk, cb_ = maskf[:, tb, :], combf[:, tb, :]
        S.op('dve', lambda e, mk=mk: e.tensor_scalar(mk, lg, m8[:, 0:1], None, ALU.is_equal), r=['lg', 'm8'], w=['maskf'])
        S.op('dve', lambda e: e.tensor_scalar(c2, lg, m8[:, 1:2], None, ALU.is_equal), r=['lg', 'm8'], w=['c2'])
        S.op('dve', lambda e, mk=mk, cb_=cb_: e.tensor_scalar(cb_, mk, sm[:, 3:4], None, ALU.mult), r=['maskf', 'sm'], w=['combf'])
        S.op('dve', lambda e, cb_=cb_: e.scalar_tensor_tensor(cb_, c2, sm[:, 4:5], cb_, ALU.mult, ALU.add), r=['c2', 'sm', 'combf'], w=['combf'])
        S.op('dve', lambda e, mk=mk: e.tensor_tensor(mk, mk, c2, ALU.add), r=['maskf', 'c2'], w=['maskf'])
        S.op('dve', lambda e, mk=mk, tb=tb: e.tensor_copy(maskb[:, tb, :], mk), r=['maskf'], w=['maskb'])
        for hf in range(2):
            def fn(e, hf=hf, ts_=ts_):
                ins = None
                for kk in range(8):
                    ins = e.transpose(psb[6 + hf][:, kk * 128:(kk + 1) * 128], uT[:, hf * 8 + kk, ts_], K.ident)
                return ins
            S.op('pe', fn, r=['uT', 'ident'], w=['ps%d' % (6 + hf)])
            S.op('act', lambda e, hf=hf, tb=tb: e.activation(u_tok[:, tb, hf * 1024:(hf + 1) * 1024], psb[6 + hf][:, 0:1024], AF.Copy),
                 r=['ps%d' % (6 + hf)], w=['u_tok'])
    for tb in range(NTB):
        K.mm_group(ps[1][:, tb * 8:(tb + 1) * 8], [K.ones] * tb + [tri], [maskb[:, j, :] for j in range(tb)] + [maskb[:, tb, :]],
                   r=['maskb', 'tri', 'ones'], w=['ps1'])
    S.op('dve', lambda e: e.tensor_copy(posf[0], ps[1][:, 0:NTB * 8].rearrange("p (a b) -> p a b", b=8)), r=['ps1'], w=['posf'])
    S.op('dve', lambda e: e.tensor_scalar(posf[1], posf[0], -512.0, None, ALU.add), r=['posf'], w=['posf'])
    K.mm_group(ps[2][:, 0:8], [K.ones] * NTB, [maskb[:, j, :] for j in range(NTB)], r=['maskb', 'ones'], w=['ps2'])
    S.op('dve', lambda e: e.tensor_copy(cntf, ps[2][:, 0:8]), r=['ps2'], w=['cntf'])
    S.op('dve', lambda e: e.tensor_copy(cnt_i, cntf), r=['cntf'], w=['cnt_i'])
    S.barrier()
    A.off = base
    SMAX = 512
    G1 = A.alloc((NTB, SMAX), BF16)
    Gc = A.alloc((NTB, SMAX), BF16)
    GcT = A.alloc((SMAX // 128, NMOE), BF16)
    ugT_off = A.off
    ugT = A.alloc((NCH, SMAX), BF16)
    accs = A.alloc((NCH, SMAX))
    accb = A.alloc((NCH, 128), BF16)
    Ys = A.at(ugT_off, (SMAX // 128, D), BF16)
    hbuf = [A.alloc((4, SMAX), BF16) for _ in range(2)]
    sbuf = [A.alloc(SMAX) for _ in range(2)]
    stg = [A.alloc(NMOE) for _ in range(2)]
    ws13 = K.WS('we13', 5, (NCH, 256))
    ws2 = K.WS('we2', 3, (2, D))
    halves = [(0, 384), (384, 384)]
    S.op('dve', lambda e: e.memset(stg[0], 0.0), w=['stg0'])
    for d in range(NCH):
        S.op('pool', lambda e, d=d: [e.dma_start(out=K.MO[:, d, :], in_=stg[0])], r=['stg0'], w=['MO%d' % d], chan='st_acc0')

    def expert_pass(ex, pidx, ns):
        nst = ns // 128
        for tb in range(NTB):
            S.op('dve', lambda e, tb=tb: e.tensor_scalar(G1[:, tb, 0:ns], iota[:, 0:ns], posf[pidx][:, tb, ex:ex + 1],
                                                         maskf[:, tb, ex:ex + 1], ALU.is_equal, ALU.mult), r=['iota'], w=['G1'])
            S.op('dve', lambda e, tb=tb: e.tensor_scalar(Gc[:, tb, 0:ns], iota[:, 0:ns], posf[pidx][:, tb, ex:ex + 1],
                                                         combf[:, tb, ex:ex + 1], ALU.is_equal, ALU.mult), r=['iota'], w=['Gc'])
        for k in range(NCH):
            b = k % 2
            K.mm_group(ps[b][:, 0:ns], [u_tok[:, tb, k * 128:(k + 1) * 128] for tb in range(NTB)],
                       [G1[:, tb, 0:ns] for tb in range(NTB)], r=['G1'], w=['ps%d' % b])
            S.op('act', lambda e, k=k, b=b: e.activation(ugT[:, k, 0:ns], ps[b][:, 0:ns], AF.Copy), r=['ps%d' % b], w=['ugT'])
        ffn_pass(K, ugT, 'ugT', [(0, ns)], I['exp_w1'][ex], I['exp_w3'][ex], I['exp_w2'][ex], DFFE, accs, 'accs',
                 ws13, ws2, hbuf, sbuf, True)
        for st_ in range(nst):
            S.op('act', lambda e, st_=st_: e.activation(accb, accs[:, :, st_ * 128:(st_ + 1) * 128], AF.Copy), r=['accs_%d' % d_ for d_ in range(NCH)], w=['accb'])
            for hf in range(2):
                def fn(e, hf=hf):
                    ins = None
                    for kk in range(8):
                        ins = e.transpose(psb[6][:, kk * 128:(kk + 1) * 128], accb[:, hf * 8 + kk, :], K.ident)
                    return ins
                S.op('pe', fn, r=['accb', 'ident'], w=['ps6'])
                S.op('act', lambda e, st_=st_, hf=hf: e.activation(Ys[:, st_, hf * 1024:(hf + 1) * 1024], psb[6][:, 0:1024], AF.Copy),
                     r=['ps6'], w=['ugT'])

            def fn2(e, st_=st_):
                ins = None
                for tb in range(NTB):
                    ins = e.transpose(psb[7][:, tb * 128:(tb + 1) * 128], Gc[:, tb, st_ * 128:(st_ + 1) * 128], K.ident)
                return ins
            S.op('pe', fn2, r=['Gc', 'ident'], w=['ps7'])
            S.op('act', lambda e, st_=st_: e.activation(GcT[:, st_, :], psb[7][:, 0:NMOE], AF.Copy), r=['ps7'], w=['GcT'])
        for d in range(NCH):
            sg, sk = stg[d % 2], 'stg%d' % (d % 2)
            for hi_, (c0, n) in enumerate(halves):
                b = 4 + hi_
                K.mm_group(ps[b][:, 0:n], [Ys[:, st_, d * 128:(d + 1) * 128] for st_ in range(nst)],
                           [GcT[:, st_, c0:c0 + n] for st_ in range(nst)], r=['ugT', 'GcT'], w=['ps%d' % b])
                if hi_ == 0:
                    S.op('act', lambda e, b=b, sg=sg, c0=c0, n=n: e.activation(sg[:, c0:c0 + n], ps[b][:, 0:n], AF.Copy),
                         r=['ps%d' % b], w=[sk])
                else:
                    S.op('dve', lambda e, b=b, sg=sg, c0=c0, n=n: e.tensor_copy(sg[:, c0:c0 + n], ps[b][:, 0:n]),
                         r=['ps%d' % b], w=[sk])
            S.op('pool', lambda e, d=d, sg=sg: [e.dma_start(out=K.MO[:, d, :], in_=sg, accum_op=ALU.add)],
                 r=[sk], w=['MO%d' % d], chan='st_acc%d' % (d % 2))

    for ex in range(NEXP):
        S.branch_begin((cnt_i[0:1, ex:ex + 1], list(MOE_THR)))
        expert_pass(ex, 0, 256)
        S.branch_next()
        expert_pass(ex, 0, 384)
        S.branch_next()
        expert_pass(ex, 0, 512)
        S.branch_next()
        expert_pass(ex, 0, 512)
        expert_pass(ex, 1, 256)
        S.branch_end()
```
